# Optimizing a Trainium2 kernel written in Bass

```python
import math
import jax
import jax.numpy as jnp
from jax import lax
import numpy as np

D_MODEL = 1024
BATCH = 8
SEQ = 2048
DEPTH = 2

GRID_W = 64
CTX_LEN = 256
F32 = jnp.float32

HY_DIM = 1024
HY_ORDER = 2
HY_BANDS = 16
HY_POS_DIM = 1 + 2 * HY_BANDS
HY_FF = 64
HY_DECAY_TARGET = 1e-2
HY_DECAY_PCT_SHORT = 0.3
HY_DECAY_PCT_LONG = 1.5
HG_HEADS = 8
HG_DIM = 1024
HG_HK = HG_DIM // HG_HEADS
HG_HV = HG_DIM // HG_HEADS
HG_CHUNK = 64
RT_HEADS = 4
RT_HK = 256
RT_HV = 512
RT_QK = RT_HEADS * RT_HK
RT_V = RT_HEADS * RT_HV
RT_CHUNK = 128
RT_ROPE_BASE = 10000.0
N_BRANCH = 3
STATE_SIZES = (HG_DIM, HG_DIM, HG_DIM, RT_QK, RT_V)
OUTPUT_SIZES = (HG_DIM, HG_DIM, RT_QK, RT_V, 3 * HY_DIM, N_BRANCH * D_MODEL)
N_STATE_COLS = 3 * HG_DIM + RT_QK + RT_V
N_IN_COLS = N_STATE_COLS + 2 * HG_DIM + RT_QK + RT_V + 3 * HY_DIM + N_BRANCH * D_MODEL
D_FF = 2816
N_EXPERTS = 8
TOP_K = 2
D_EXPERT = 3584
MOE_BLOCK = 256
N_DENSE = (DEPTH + 1) // 2
N_MOE = DEPTH // 2
DN_ALPHA = (2 * DEPTH) ** 0.25
DN_BETA = (8 * DEPTH) ** -0.25
LN_EPS = 1e-5

kernel_name = 'hybrid_hyena_hgrn2_retention_moe_dit_trunk'


def _split(x, sizes):
    return jnp.split(x, np.cumsum(sizes)[:-1].tolist(), axis=-1)


def _layer_norm(x, g, b):
    xf = x.astype(F32)
    mu = jnp.mean(xf, axis=-1, keepdims=True)
    var = jnp.mean(jnp.square(xf - mu), axis=-1, keepdims=True)
    return ((xf - mu) * lax.rsqrt(var + LN_EPS)).astype(x.dtype) * g + b


def _rms_norm(x, w=None):
    xf = x.astype(F32)
    y = (xf * lax.rsqrt(jnp.mean(jnp.square(xf), axis=-1, keepdims=True) + LN_EPS)).astype(x.dtype)
    return y if w is None else y * w


def _heads(x, n):
    b, l, _ = x.shape
    return x.reshape(b, l, n, -1).transpose(0, 2, 1, 3)


def _merge_heads(x):
    b, h, l, d = x.shape
    return x.transpose(0, 2, 1, 3).reshape(b, l, h * d)


def _to_chunks(x, c):
    b, h, l, d = x.shape
    return jnp.moveaxis(x.reshape(b, h, l // c, c, d), 2, 0)


def _from_chunks(x):
    n, b, h, c, d = x.shape
    return jnp.moveaxis(x, 0, 2).reshape(b, h, n * c, d)


def _flip_seq(args):
    return tuple(None if a is None else jnp.flip(a, axis=2) for a in args)


def _hyena_filters(L, w1, b1, freq, w2, b2, w3):
    t01 = jnp.linspace(0.0, 1.0, L, dtype=F32)[:, None]
    ang = 2.0 * math.pi * jnp.arange(L, dtype=F32)[:, None] / L
    bands = jnp.linspace(1e-4, HY_BANDS - 1, HY_BANDS, dtype=F32)[None, :]
    z = jnp.concatenate([t01, jnp.cos(bands * ang), -jnp.sin(bands * ang)], axis=-1).astype(w1.dtype)
    hdn = jnp.sin(freq[0] * (z @ w1 + b1))
    hdn = jnp.sin(freq[1] * (hdn @ w2 + b2))
    filt = (hdn @ w3).reshape(L, HY_ORDER, 2, HY_DIM)
    deltas = jnp.abs(jnp.linspace(math.log(HY_DECAY_TARGET) / HY_DECAY_PCT_LONG,
                                  math.log(HY_DECAY_TARGET) / HY_DECAY_PCT_SHORT, HY_DIM, dtype=F32))
    filt = filt * jnp.exp(-t01[:, :, None, None] * deltas).astype(filt.dtype)
    fwd = filt[:, :, 0]
    bwd = jnp.flip(filt[1:, :, 1], axis=0)
    kern = jnp.concatenate([fwd, jnp.zeros((1, HY_ORDER, HY_DIM), filt.dtype), bwd], axis=0)
    return kern / jnp.sum(jnp.abs(kern), axis=0, keepdims=True)


def _fft_long_conv(u, filt):
    L = u.shape[1]
    U = jnp.fft.rfft(u.astype(F32), n=2 * L, axis=1)
    K = jnp.fft.rfft(filt.astype(F32), axis=0)
    return jnp.fft.irfft(U * K[None], n=2 * L, axis=1)[:, :L].astype(u.dtype)


def _hyena(u, lp, grid):
    b, L, c3 = u.shape
    w = lp['hy_conv_w'].astype(u.dtype)
    if grid:
        rows = L // GRID_W
        u4 = u.reshape(b, rows, GRID_W, c3)
        filt = w[:, :, None, :]
    else:
        u4 = u[:, None]
        filt = w[1:2, :, None, :]
    uc = lax.conv_general_dilated(u4, filt, (1, 1), 'SAME',
                                  dimension_numbers=('NHWC', 'HWIO', 'NHWC'),
                                  feature_group_count=c3).reshape(b, L, c3) + lp['hy_conv_b']
    v, x1, x2 = jnp.split(uc, 3, axis=-1)
    filters = _hyena_filters(L, lp['hy_ff_w1'], lp['hy_ff_b1'], lp['hy_ff_freq'],
                             lp['hy_ff_w2'], lp['hy_ff_b2'], lp['hy_ff_w3'])
    z = v
    for n, gate in enumerate((x1, x2)):
        z = gate * (_fft_long_conv(z, filters[:, n]) + lp['hy_skip'][n] * z)
    return z


def _gla_scan(q, k, v, log_f, s0, with_output):
    C = HG_CHUNK
    mask = jnp.tril(jnp.ones((C, C), dtype=bool))
    ks, vs, gs = _to_chunks(k, C), _to_chunks(v, C), _to_chunks(log_f, C)

    def update(S, kc, vc, bcum):
        b_end = bcum[:, :, -1:, :]
        return (jnp.exp(b_end[:, :, 0, :, None]) * S
                + jnp.einsum('bhsk,bhsv->bhkv', kc * jnp.exp(b_end - bcum), vc))

    if with_output:
        qs = _to_chunks(q, C)

        def step(S, inp):
            qc, kc, vc, gc = inp
            bcum = jnp.cumsum(gc.astype(F32), axis=2)
            o_inter = jnp.einsum('bhtk,bhkv->bhtv', qc * jnp.exp(bcum), S)
            diff = bcum[:, :, :, None, :] - bcum[:, :, None, :, :]
            decay = jnp.where(mask[:, :, None], jnp.exp(jnp.minimum(diff, 0.0)), 0.0)
            scores = jnp.einsum('bhtk,bhtsk->bhts', qc, decay * kc[:, :, None, :, :])
            o = o_inter + jnp.einsum('bhts,bhsv->bhtv', scores, vc)
            return update(S, kc, vc, bcum), o.astype(v.dtype)

        S, os = lax.scan(step, s0, (qs, ks, vs, gs))
        return _from_chunks(os), S

    def step_state(S, inp):
        kc, vc, gc = inp
        return update(S, kc, vc, jnp.cumsum(gc.astype(F32), axis=2)), None

    S, _ = lax.scan(step_state, s0, (ks, vs, gs))
    return None, S


def _retention_scan(q, k, v, log_gamma, s0, with_output):
    C = RT_CHUNK
    pos = jnp.arange(C, dtype=F32)
    lg = log_gamma.astype(F32)
    rel = pos[:, None] - pos[None, :]
    intra = jnp.where(rel >= 0, jnp.exp(jnp.maximum(rel, 0.0)[None] * lg[:, None, None]), 0.0)
    q_decay = jnp.exp((pos + 1.0)[None, :] * lg[:, None])[None, :, :, None]
    k_decay = jnp.exp((C - 1.0 - pos)[None, :] * lg[:, None])[None, :, :, None]
    s_decay = jnp.exp(C * lg)[None, :, None, None]

    def update(S, kc, vc):
        return s_decay * S + jnp.einsum('bhsk,bhsv->bhkv', kc * k_decay, vc)

    ks, vs = _to_chunks(k, C), _to_chunks(v, C)
    if with_output:
        qs = _to_chunks(q, C)

        def step(S, inp):
            qc, kc, vc = inp
            scores = jnp.einsum('bhtk,bhsk->bhts', qc, kc) * intra
            o = (jnp.einsum('bhtk,bhkv->bhtv', qc, S) * q_decay
                 + jnp.einsum('bhts,bhsv->bhtv', scores, vc))
            return update(S, kc, vc), o.astype(v.dtype)

        S, os = lax.scan(step, s0, (qs, ks, vs))
        return _from_chunks(os), S

    def step_state(S, inp):
        kc, vc = inp
        return update(S, kc, vc), None

    S, _ = lax.scan(step_state, s0, (ks, vs))
    return None, S


def _run_dirs(scan_fn, lat_in, ctx_in, s0, need_ctx_out):
    o_lat, o_ctx = None, None
    for d in range(2):
        o_c, s_c = scan_fn(*ctx_in[d], s0, need_ctx_out)
        o_l, _ = scan_fn(*lat_in[d], s_c, True)
        if d == 1:
            o_l = jnp.flip(o_l, axis=2)
            o_c = None if o_c is None else jnp.flip(o_c, axis=2)
        o_lat = o_l if o_lat is None else o_lat + o_l
        if need_ctx_out:
            o_ctx = o_c if o_ctx is None else o_ctx + o_c
    return o_lat, o_ctx


def _hgrn2_gates(f_logit, lb):
    lbb = lb[None, :, None, :]
    fl = f_logit.astype(F32)
    log_f = jnp.logaddexp(jnp.log(lbb), jnp.log1p(-lbb) + jax.nn.log_sigmoid(fl))
    key = ((1.0 - lbb) * jax.nn.sigmoid(-fl)).astype(f_logit.dtype)
    return log_f, key


def _hgrn2(lat, ctx, lb, norm_w, need_ctx_out):
    def prep(f_fwd, f_bwd, i, q):
        vh = _heads(i, HG_HEADS)
        qh = None if q is None else _heads(jax.nn.silu(q), HG_HEADS) * HG_HK ** -0.5
        dirs = []
        for d, f in enumerate((f_fwd, f_bwd)):
            log_f, key = _hgrn2_gates(_heads(f, HG_HEADS), lb[d].reshape(HG_HEADS, HG_HK))
            args = (qh, key, vh, log_f)
            dirs.append(args if d == 0 else _flip_seq(args))
        return dirs

    b = lat[0].shape[0]
    s0 = jnp.zeros((b, HG_HEADS, HG_HK, HG_HV), F32)
    o_l, o_c = _run_dirs(_gla_scan, prep(*lat[:4]), prep(*ctx[:4]), s0, need_ctx_out)

    def readout(o, g):
        return _rms_norm(_merge_heads(o), norm_w) * jax.nn.silu(g)

    return readout(o_l, lat[4]), (readout(o_c, ctx[4]) if need_ctx_out else None)


def _rotary(x, pos):
    half = x.shape[-1] // 2
    inv = 1.0 / (RT_ROPE_BASE ** jnp.linspace(0.0, 1.0, half, dtype=F32))
    ang = pos[:, None] * inv[None, :]
    cos, sin = jnp.cos(ang).astype(x.dtype), jnp.sin(ang).astype(x.dtype)
    x1, x2 = x[..., :half], x[..., half:]
    return jnp.concatenate([x1 * cos - x2 * sin, x1 * sin + x2 * cos], axis=-1)


def _retention_log_gammas():
    j = jnp.arange(2 * RT_HEADS, dtype=F32)
    lg = jnp.log1p(-jnp.exp2(-5.0 - j))
    return lg[0::2], lg[1::2]


def _retention(lat, ctx, need_ctx_out):
    lg_f, lg_b = _retention_log_gammas()

    def prep(q, k, v, pos):
        kh = _rotary(_heads(k, RT_HEADS), pos) * RT_HK ** -0.5
        qh = None if q is None else _rotary(_heads(q, RT_HEADS), pos)
        vh = _heads(v, RT_HEADS)
        return [(qh, kh, vh, lg_f), _flip_seq((qh, kh, vh)) + (lg_b,)]

    b, L = lat[1].shape[0], lat[1].shape[1]
    Lc = ctx[1].shape[1]
    pos_c = jnp.arange(Lc, dtype=F32)
    pos_l = Lc + jnp.arange(L, dtype=F32)
    s0 = jnp.zeros((b, RT_HEADS, RT_HK, RT_HV), F32)
    o_l, o_c = _run_dirs(_retention_scan, prep(lat[0], lat[1], lat[2], pos_l),
                         prep(ctx[0], ctx[1], ctx[2], pos_c), s0, need_ctx_out)

    def readout(o, g):
        return _merge_heads(_rms_norm(o)) * jax.nn.silu(g)

    return readout(o_l, lat[3]), (readout(o_c, ctx[3]) if need_ctx_out else None)


def _merge(y_hy, y_hg, y_rt, gate_logits, lp):
    g_hy, g_hg, g_rt = jnp.split(jax.nn.sigmoid(gate_logits), N_BRANCH, axis=-1)
    m = g_hy * (y_hy @ lp['p_hy']) + g_hg * (y_hg @ lp['p_hg']) + g_rt * (y_rt @ lp['p_rt'])
    return m @ lp['w_o']


def _mixer(h, hc, lp, lb, need_ctx_out):
    w_in = lp['w_in']
    f_f, f_b, hi, rk, rv, hq, hg, rq, rg, hy, br = _split(h @ w_in, STATE_SIZES + OUTPUT_SIZES)
    if need_ctx_out:
        cf_f, cf_b, chi, crk, crv, chq, chg, crq, crg, chy, cbr = _split(hc @ w_in, STATE_SIZES + OUTPUT_SIZES)
    else:
        cf_f, cf_b, chi, crk, crv = _split(hc @ w_in[:, :N_STATE_COLS], STATE_SIZES)
        chq = chg = crq = crg = chy = cbr = None
    y_hg, c_hg = _hgrn2((f_f, f_b, hi, hq, hg), (cf_f, cf_b, chi, chq, chg), lb, lp['hg_norm_w'], need_ctx_out)
    y_rt, c_rt = _retention((rq, rk, rv, rg), (crq, crk, crv, crg), need_ctx_out)
    y_hy = _hyena(hy, lp, True)
    m = _merge(y_hy, y_hg, y_rt, br, lp)
    if not need_ctx_out:
        return m, None
    c_hy = _hyena(chy, lp, False)
    return m, _merge(c_hy, c_hg, c_rt, cbr, lp)


def _swiglu(h, w1, w3, w2):
    return (jax.nn.silu(h @ w1) * (h @ w3)) @ w2


def _moe_swiglu(t, router, w1, w3, w2):
    n, d = t.shape
    e = router.shape[-1]
    logits = (t @ router).astype(F32)
    top_val, top_idx = lax.top_k(logits, TOP_K)
    gate = jax.nn.softmax(top_val, axis=-1).astype(t.dtype)
    flat_e = top_idx.reshape(-1)
    flat_t = jnp.repeat(jnp.arange(n, dtype=jnp.int32), TOP_K)
    flat_g = gate.reshape(-1)
    order = jnp.argsort(flat_e)
    se, st, sg = flat_e[order], flat_t[order], flat_g[order]
    counts = jnp.bincount(flat_e, length=e)
    starts = jnp.cumsum(counts) - counts
    padded = (counts + MOE_BLOCK - 1) // MOE_BLOCK * MOE_BLOCK
    pad_end = jnp.cumsum(padded)
    dest = (pad_end - padded)[se] + jnp.arange(n * TOP_K) - starts[se]
    n_blocks = -(-(n * TOP_K) // MOE_BLOCK) + e
    n_slots = n_blocks * MOE_BLOCK
    slot_tok = jnp.full((n_slots,), n, dtype=jnp.int32).at[dest].set(st)
    slot_g = jnp.zeros((n_slots,), t.dtype).at[dest].set(sg)
    block_start = jnp.arange(n_blocks) * MOE_BLOCK
    block_e = jnp.minimum(jnp.sum(block_start[:, None] >= pad_end[None, :], axis=1), e - 1)
    xb = jnp.concatenate([t, jnp.zeros((1, d), t.dtype)], axis=0)[slot_tok].reshape(n_blocks, MOE_BLOCK, d)

    def run_block(args):
        xblk, eid = args
        return (jax.nn.silu(xblk @ w1[eid]) * (xblk @ w3[eid])) @ w2[eid]

    yb = lax.map(run_block, (xb, block_e)).reshape(n_slots, d)
    y = jnp.zeros((n + 1, d), t.dtype).at[slot_tok].add(yb * slot_g[:, None])
    return y[:n]


def _layer(x, xc, c, c_ctx, lp, lb, ffn_params, use_moe, need_ctx_out):
    b, l, d = x.shape
    mod = jax.nn.silu(c) @ lp['ada_w'] + lp['ada_b']
    mod_c = jax.nn.silu(c_ctx) @ lp['ada_w'] + lp['ada_b']
    sh1, sc1, gt1, sh2, sc2, gt2 = [m[:, None, :] for m in jnp.split(mod, 6, axis=-1)]
    sh1c, sc1c, gt1c, sh2c, sc2c, gt2c = jnp.split(mod_c, 6, axis=-1)
    h = x * (1.0 + sc1) + sh1
    hc = xc * (1.0 + sc1c) + sh1c
    m, mc = _mixer(h, hc, lp, lb, need_ctx_out)
    x = _layer_norm(DN_ALPHA * x + gt1 * m, lp['ln1_g'], lp['ln1_b'])
    h2 = x * (1.0 + sc2) + sh2
    if need_ctx_out:
        xc = _layer_norm(DN_ALPHA * xc + gt1c * mc, lp['ln1_g'], lp['ln1_b'])
        h2c = xc * (1.0 + sc2c) + sh2c
    if use_moe:
        tok = h2.reshape(b * l, d)
        if need_ctx_out:
            tok = jnp.concatenate([tok, h2c.reshape(-1, d)], axis=0)
        f_all = _moe_swiglu(tok, *ffn_params)
        f = f_all[:b * l].reshape(b, l, d)
        fc = f_all[b * l:].reshape(xc.shape) if need_ctx_out else None
    else:
        f = _swiglu(h2, *ffn_params)
        fc = _swiglu(h2c, *ffn_params) if need_ctx_out else None
    x = _layer_norm(DN_ALPHA * x + gt2 * f, lp['ln2_g'], lp['ln2_b'])
    xc = _layer_norm(DN_ALPHA * xc + gt2c * fc, lp['ln2_g'], lp['ln2_b']) if need_ctx_out else None
    return x, xc


def setup_inputs(seed: int = 0) -> dict:
    key = jax.random.key(seed)
    keys = iter(jax.random.split(key, 48))

    def nrm(shape, scale):
        return jax.random.normal(next(keys), shape, F32) * scale

    D = D_MODEL
    return {
        'x': nrm((BATCH, SEQ, D), 1.0),
        'c': nrm((BATCH, D), 1.0),
        'ctx': nrm((BATCH, CTX_LEN, D), 1.0),
        'c_ctx': nrm((D,), 1.0),
        'ada_w': nrm((DEPTH, D, 6 * D), D ** -0.5),
        'ada_b': nrm((DEPTH, 6 * D), 0.02),
        'w_in': nrm((DEPTH, D, N_IN_COLS), D ** -0.5),
        'hy_conv_w': nrm((DEPTH, 3, 3, 3 * HY_DIM), 1.0 / 3.0),
        'hy_conv_b': nrm((DEPTH, 3 * HY_DIM), 0.02),
        'hy_ff_w1': nrm((DEPTH, HY_POS_DIM, HY_FF), HY_POS_DIM ** -0.5),
        'hy_ff_b1': nrm((DEPTH, HY_FF), 0.1),
        'hy_ff_freq': 1.0 + nrm((DEPTH, 2, HY_FF), 0.02),
        'hy_ff_w2': nrm((DEPTH, HY_FF, HY_FF), HY_FF ** -0.5),
        'hy_ff_b2': nrm((DEPTH, HY_FF), 0.1),
        'hy_ff_w3': nrm((DEPTH, HY_FF, HY_ORDER * 2 * HY_DIM), HY_FF ** -0.5),
        'hy_skip': nrm((DEPTH, HY_ORDER, HY_DIM), 0.5),
        'hg_lb_logits': nrm((2, DEPTH, HG_DIM), 0.1),
        'hg_norm_w': 1.0 + nrm((DEPTH, HG_DIM), 0.02),
        'p_hy': nrm((DEPTH, HY_DIM, D), HY_DIM ** -0.5),
        'p_hg': nrm((DEPTH, HG_DIM, D), HG_DIM ** -0.5),
        'p_rt': nrm((DEPTH, RT_V, D), RT_V ** -0.5),
        'w_o': nrm((DEPTH, D, D), D ** -0.5 * DN_BETA),
        'ln1_g': 1.0 + nrm((DEPTH, D), 0.02),
        'ln1_b': nrm((DEPTH, D), 0.02),
        'ln2_g': 1.0 + nrm((DEPTH, D), 0.02),
        'ln2_b': nrm((DEPTH, D), 0.02),
        'ffn_w1': nrm((N_DENSE, D, D_FF), D ** -0.5),
        'ffn_w3': nrm((N_DENSE, D, D_FF), D ** -0.5),
        'ffn_w2': nrm((N_DENSE, D_FF, D), D_FF ** -0.5 * DN_BETA),
        'moe_router': nrm((N_MOE, D, N_EXPERTS), D ** -0.5),
        'moe_w1': nrm((N_MOE, N_EXPERTS, D, D_EXPERT), D ** -0.5),
        'moe_w3': nrm((N_MOE, N_EXPERTS, D, D_EXPERT), D ** -0.5),
        'moe_w2': nrm((N_MOE, N_EXPERTS, D_EXPERT, D), D_EXPERT ** -0.5 * DN_BETA),
    }


def reference(x, c, ctx, c_ctx, ada_w, ada_b, w_in, hy_conv_w, hy_conv_b, hy_ff_w1, hy_ff_b1,
              hy_ff_freq, hy_ff_w2, hy_ff_b2, hy_ff_w3, hy_skip, hg_lb_logits, hg_norm_w,
              p_hy, p_hg, p_rt, w_o, ln1_g, ln1_b, ln2_g, ln2_b, ffn_w1, ffn_w3, ffn_w2,
              moe_router, moe_w1, moe_w3, moe_w2):
    cs = jnp.cumsum(jax.nn.softmax(hg_lb_logits.astype(F32), axis=1), axis=1)
    lower_bounds = cs - cs[:, :1]
    xc = ctx
    for i in range(DEPTH):
        lp = {
            'ada_w': ada_w[i], 'ada_b': ada_b[i], 'w_in': w_in[i],
            'hy_conv_w': hy_conv_w[i], 'hy_conv_b': hy_conv_b[i],
            'hy_ff_w1': hy_ff_w1[i], 'hy_ff_b1': hy_ff_b1[i], 'hy_ff_freq': hy_ff_freq[i],
            'hy_ff_w2': hy_ff_w2[i], 'hy_ff_b2': hy_ff_b2[i], 'hy_ff_w3': hy_ff_w3[i],
            'hy_skip': hy_skip[i], 'hg_norm_w': hg_norm_w[i],
            'p_hy': p_hy[i], 'p_hg': p_hg[i], 'p_rt': p_rt[i], 'w_o': w_o[i],
            'ln1_g': ln1_g[i], 'ln1_b': ln1_b[i], 'ln2_g': ln2_g[i], 'ln2_b': ln2_b[i],
        }
        g = i // 2
        if i % 2 == 0:
            ffn_params = (ffn_w1[g], ffn_w3[g], ffn_w2[g])
        else:
            ffn_params = (moe_router[g], moe_w1[g], moe_w3[g], moe_w2[g])
        x, xc = _layer(x, xc, c, c_ctx, lp, lower_bounds[:, i], ffn_params, i % 2 == 1, i < DEPTH - 1)
    return x
```

```python
import math
import numpy as np
import ml_dtypes
import concourse.bass as bass
import concourse.mybir as mybir
from concourse.bass_utils import run_bass_kernel_spmd

F32 = mybir.dt.float32
BF16 = mybir.dt.bfloat16
AF = mybir.ActivationFunctionType
ALU = mybir.AluOpType

D = 1024
L = 2048
LC = 256
T = L + LC
NT = T // 128
DEPTH = 2
NCOL = 17408
D_FF = 2816
NEXP = 8
D_EXP = 3584
EPS = 1e-5
ALPHA = (2 * DEPTH) ** 0.25
C_FF, C_FB, C_HI, C_RK, C_RV = 0, 1024, 2048, 3072, 4096
C_HQ, C_HG, C_RQ, C_RG, C_HY, C_BR = 6144, 7168, 8192, 9216, 11264, 14336
TWO_PI = 2.0 * math.pi


class Res:
    __slots__ = ("w", "rs")

    def __init__(self):
        self.w = None
        self.rs = {}


class Eng:
    def __init__(self, nc, name, eng):
        self.name = name
        self.eng = eng
        self.sem = nc.alloc_semaphore(name="sem_" + name)
        self.cnt = 0
        self.waited = {}


class KB:
    def __init__(self, nc):
        self.nc = nc
        self.E = {n: Eng(nc, n, getattr(nc, n)) for n in ("tensor", "vector", "scalar", "gpsimd", "sync")}
        self.res = {}
        self.slots = {}
        for q in ("sync", "gpsimd"):
            self.slots[q] = [[nc.alloc_semaphore(name=f"dq_{q}{i}"), 0] for i in range(12)]
        self.slot_i = {"sync": 0, "gpsimd": 0}
        self.n_sb = 0
        self.sb_off = (nc.sbuf_base + 63) // 64 * 64
        self.sb_top = nc.sbuf_top
        self.psum = [nc.alloc_psum_tensor(f"ps{i}", [128, 512], F32) for i in range(8)]
        self.ps_i = 0

    def sb(self, shape, dt, name=None):
        self.n_sb += 1
        nbytes = int(np.prod(shape[1:])) * (2 if dt == BF16 else 4)
        nbytes = (nbytes + 63) // 64 * 64
        off = self.sb_off
        assert off + nbytes <= self.sb_top, f"SBUF overflow {off}+{nbytes} > {self.sb_top}"
        self.sb_off += nbytes
        return self.nc.alloc_sbuf_tensor_at((name or "sb") + f"_{self.n_sb}", list(shape), dt, offset=off)

    def mark(self):
        return self.sb_off

    def release(self, m):
        self.barrier()
        self.sb_off = m

    def barrier(self):
        tks = [(e.sem, e.cnt) for e in self.E.values() if e.cnt > 0]
        for q in self.slots:
            tks += [(sl[0], sl[1]) for sl in self.slots[q] if sl[1] > 0]
        for e in self.E.values():
            for tk in tks:
                self._wait(e, tk)

    def dram(self, name, shape, dt):
        kind = "ExternalOutput" if name in getattr(self, "dbg", ()) else "Internal"
        return self.nc.dram_tensor(name, list(shape), dt, kind=kind).ap()

    def ps(self):
        pool = getattr(self, "ps_pool", None) or list(range(8))
        p = self.psum[pool[self.ps_i % len(pool)]]
        self.ps_i += 1
        return p

    def _r(self, ap):
        if isinstance(ap, Res):
            return ap
        n = ap.tensor.name
        r = self.res.get(n)
        if r is None:
            r = self.res[n] = Res()
        return r

    def _wait(self, E, tk):
        if tk is None:
            return
        sem, val = tk
        k = id(sem)
        if E.waited.get(k, (None, 0))[1] >= val:
            return
        E.waited[k] = (sem, val)
        E.eng.wait_ge(sem, val)

    def _deps(self, E, ins, outs):
        for a in ins:
            self._wait(E, self._r(a).w)
        for a in outs:
            r = self._r(a)
            self._wait(E, r.w)
            for tk in r.rs.values():
                self._wait(E, tk)

    def _commit(self, tk, ins, outs):
        for a in ins:
            self._r(a).rs[id(tk[0])] = tk
        for a in outs:
            r = self._r(a)
            r.w = tk
            r.rs = {}

    def op(self, en, fn, outs, ins):
        E = self.E[en]
        self._deps(E, ins, outs)
        inst = fn(E.eng)
        E.cnt += 1
        inst.then_inc(E.sem, 1)
        tk = (E.sem, E.cnt)
        self._commit(tk, ins, outs)
        return tk

    def dma(self, q, out, in_, extra_ins=(), extra_outs=(), **kw):
        E = self.E[q]
        sl = self.slots[q][self.slot_i[q] % len(self.slots[q])]
        self.slot_i[q] += 1
        if sl[1] > 0:
            self._wait(E, (sl[0], sl[1]))
        ins = [in_] + list(extra_ins)
        outs = [out] + list(extra_outs)
        self._deps(E, ins, outs)
        inst = E.eng.dma_start(out=out, in_=in_, **kw)
        sl[1] += 16
        inst.then_inc(sl[0], 16)
        tk = (sl[0], sl[1])
        self._commit(tk, ins, outs)
        return tk

    def mm(self, out, lhsT, rhs, start=True, stop=True, **kw):
        return self.op("tensor", lambda e: e.matmul(out, lhsT=lhsT, rhs=rhs, start=start, stop=stop, **kw), [out], [lhsT, rhs])

    def tr(self, out, in_, ident):
        return self.op("tensor", lambda e: e.transpose(out, in_, ident), [out], [in_, ident])

    def act(self, out, in_, func, scale=1.0, bias=0.0, eng="scalar"):
        ins = [in_] + [a for a in (scale, bias) if not isinstance(a, (int, float))]
        return self.op("scalar", lambda e: e.activation(out=out, in_=in_, func=func, scale=scale, bias=bias), [out], ins)

    def tt(self, out, in0, in1, op, eng="vector"):
        return self.op(eng, lambda e: e.tensor_tensor(out=out, in0=in0, in1=in1, op=op), [out], [in0, in1])

    def ts(self, out, in0, s1, op0, s2=None, op1=None, eng="vector"):
        ins = [in0] + [a for a in (s1, s2) if a is not None and not isinstance(a, (int, float))]
        if op1 is None:
            return self.op(eng, lambda e: e.tensor_scalar(out=out, in0=in0, scalar1=s1, scalar2=None, op0=op0), [out], ins)
        return self.op(eng, lambda e: e.tensor_scalar(out=out, in0=in0, scalar1=s1, scalar2=s2, op0=op0, op1=op1), [out], ins)

    def stt(self, out, in0, scalar, in1, op0, op1):
        ins = [in0, in1] + ([] if isinstance(scalar, (int, float)) else [scalar])
        return self.op("vector", lambda e: e.scalar_tensor_tensor(out=out, in0=in0, scalar=scalar, in1=in1, op0=op0, op1=op1), [out], ins)

    def cp(self, out, in_, eng="vector"):
        if eng == "scalar":
            return self.act(out, in_, AF.Copy)
        return self.op(eng, lambda e: e.tensor_copy(out=out, in_=in_), [out], [in_])

    def memset(self, ap, val, eng="vector"):
        return self.op(eng, lambda e: e.memset(ap, val), [ap], [])

    def finish(self):
        E = self.E["sync"]
        for q in self.slots:
            for sl in self.slots[q]:
                if sl[1] > 0:
                    self._wait(E, (sl[0], sl[1]))
        for n, e2 in self.E.items():
            if n != "sync" and e2.cnt > 0:
                self._wait(E, (e2.sem, e2.cnt))


def _bf(a):
    return np.ascontiguousarray(a.astype(ml_dtypes.bfloat16))


def dft_mats(Ls):
    N = 2 * Ls
    s = np.arange(Ls, dtype=np.int64)[:, None]
    f = np.arange(Ls, dtype=np.int64)[None, :]
    ang = 2.0 * np.pi * ((s * f) % N).astype(np.float64) / N
    Fm = np.empty((Ls, N), np.float64)
    Fm[:, :Ls] = np.cos(ang)
    Fm[:, Ls:] = np.sin(ang)
    Fm[:, Ls] = np.cos(np.pi * s[:, 0])
    nst, nft = Ls // 128, N // 128
    nblk = nft // 4
    half = nft // 2
    Fh = np.empty((nblk, 128, nst, 512), np.float32)
    Gh = np.empty((nblk, 128, 4, Ls), np.float32)
    for b in range(nblk):
        tiles = [2 * b, 2 * b + 1, half + 2 * b, half + 2 * b + 1]
        for i, ft in enumerate(tiles):
            blk = Fm[:, ft * 128:(ft + 1) * 128]
            Fh[b, :, :, i * 128:(i + 1) * 128] = blk.reshape(nst, 128, 128).transpose(1, 0, 2)
            Gh[b, :, i, :] = blk.T
    return _bf(Fh.reshape(nblk, 128, nst * 512)), _bf(Gh.reshape(nblk, 128, 4 * Ls))


def hy_pos_feats(Ls):
    t01 = np.linspace(0.0, 1.0, Ls, dtype=np.float32)[:, None]
    ang = (2.0 * np.pi * np.arange(Ls, dtype=np.float32)[:, None] / Ls).astype(np.float32)
    bands = np.linspace(1e-4, 15, 16, dtype=np.float32)[None, :]
    z = np.concatenate([t01, np.cos(bands * ang), -np.sin(bands * ang)], axis=-1).astype(np.float32)
    deltas = np.abs(np.linspace(math.log(1e-2) / 1.5, math.log(1e-2) / 0.3, 1024, dtype=np.float32))
    dec = np.exp(-t01 * deltas[None, :]).astype(np.float32)
    return np.ascontiguousarray(z.T), dec


def make_consts():
    c = {}
    c["ident_f"] = np.eye(128, dtype=np.float32)
    c["ident_b"] = _bf(np.eye(128, dtype=np.float32))
    c["Fh2048"], c["Gh2048"] = dft_mats(2048)
    c["Fh256"], c["Gh256"] = dft_mats(256)
    c["zT2048"], c["dec2048"] = hy_pos_feats(2048)
    c["zT256"], c["dec256"] = hy_pos_feats(256)
    inv = 1.0 / (10000.0 ** np.linspace(0.0, 1.0, 128, dtype=np.float32))
    ang = (np.arange(T, dtype=np.float32)[None, :] * inv[:, None].astype(np.float32)).astype(np.float32)
    c["rope"] = np.stack([np.cos(ang), np.sin(ang), np.cos(ang) / 16.0, np.sin(ang) / 16.0]).astype(np.float32)
    j = np.arange(8, dtype=np.float64)
    lg = np.log1p(-np.exp2(-5.0 - j))
    lgf, lgb = lg[0::2], lg[1::2]
    pos = np.arange(128, dtype=np.float64)
    rel = pos[None, :] - pos[:, None]
    rmask = np.zeros((4, 128, 128), np.float64)
    rq = np.zeros((4, 2, 128, 128), np.float64)
    rk = np.zeros((128, 4, 2), np.float64)
    for h in range(4):
        rmask[h] = np.where(rel >= 0, np.exp(np.maximum(rel, 0) * lgf[h]), 0.0) + np.where(rel <= 0, np.exp(np.maximum(-rel, 0) * lgb[h]), 0.0)
        rq[h, 0] = np.exp((pos + 1.0) * lgf[h])[None, :]
        rq[h, 1] = np.exp((128.0 - pos) * lgb[h])[None, :]
        rk[:, h, 0] = np.exp((127.0 - pos) * lgf[h])
        rk[:, h, 1] = np.exp(pos * lgb[h])
    c["rmask"] = rmask.astype(np.float32)
    c["rq"] = rq.astype(np.float32)
    c["rk"] = rk.astype(np.float32)
    c["rsdec"] = np.stack([np.exp(128.0 * lgf), np.exp(128.0 * lgb)]).astype(np.float32)
    same = (pos[:, None] // 32) == (pos[None, :] // 32)
    gm = np.zeros((2, 128, 128), np.float32)
    gm[0] = (same & (rel >= 0)).astype(np.float32)
    gm[1] = (same & (rel <= 0)).astype(np.float32)
    c["gmask"] = gm
    sm = np.ones((128, T), np.float32)
    sm[:, 0::32] = 0.0
    c["scanmask"] = sm
    sel = np.zeros((8, 8, 128), np.float32)
    for e in range(8):
        sel[e, e, :] = 1.0
    c["sel8"] = sel.reshape(8, 1024)
    c["ones_f"] = np.ones((128, 128), np.float32)
    c["ones_b"] = _bf(np.ones((128, 128), np.float32))
    return c


CONST_DT = {"ident_b": BF16, "Fh2048": BF16, "Gh2048": BF16, "Fh256": BF16, "Gh256": BF16, "ones_b": BF16}

W_SHAPES = {
    "ada_w": (DEPTH, D, 6 * D), "ada_b": (DEPTH, 6 * D), "w_in": (DEPTH, D, NCOL),
    "hy_conv_w": (DEPTH, 9, 3 * D), "hy_conv_b": (DEPTH, 3 * D),
    "hy_ff_w1": (DEPTH, 33, 64), "hy_ff_b1": (DEPTH, 64), "hy_ff_freq": (DEPTH, 2, 64),
    "hy_ff_w2": (DEPTH, 64, 64), "hy_ff_b2": (DEPTH, 64), "hy_ff_w3": (DEPTH, 64, 4096),
    "hy_skip": (DEPTH, 2, D), "hg_lb_logits": (2, DEPTH, D), "hg_norm_w": (DEPTH, D),
    "p_hy": (DEPTH, D, D), "p_hg": (DEPTH, D, D), "p_rt": (DEPTH, 2 * D, D), "w_o": (DEPTH, D, D),
    "ln1_g": (DEPTH, D), "ln1_b": (DEPTH, D), "ln2_g": (DEPTH, D), "ln2_b": (DEPTH, D),
    "ffn_w1": (1, D, D_FF), "ffn_w3": (1, D, D_FF), "ffn_w2": (1, D_FF, D),
    "moe_router": (1, D, NEXP), "moe_w1": (1, NEXP, D, D_EXP), "moe_w3": (1, NEXP, D, D_EXP),
    "moe_w2": (1, NEXP, D_EXP, D),
}


class Prog:
    def __init__(self, consts, stop_after=None, dbg=(), skip=()):
        self.nc = nc = bass.Bass("TRN2", target_bir_lowering=False)
        self.kb = kb = KB(nc)
        self.stop_after = stop_after
        self.dbg = set(dbg)
        kb.dbg = self.dbg
        self.ins = {}
        def inp(name, shape, dt=F32):
            self.ins[name] = nc.dram_tensor(name, list(shape), dt, kind="ExternalInput").ap()
            return self.ins[name]
        inp("x", (L, D)); inp("ctx", (LC, D)); inp("c2", (2, D))
        for n, s in W_SHAPES.items():
            if n not in skip:
                inp(n, s)
        for n, v in consts.items():
            inp(n, v.shape, CONST_DT.get(n, F32))
        self.out = nc.dram_tensor("out", [L, D], F32, kind="ExternalOutput").ap()
        self.dbg_out = {}

    def dbg_tensor(self, name, shape, dt=F32):
        kind = "ExternalOutput" if name in self.dbg else "Internal"
        t = self.nc.dram_tensor(name, list(shape), dt, kind=kind).ap()
        if name in self.dbg:
            self.dbg_out[name] = t
        return t

    def setup(self):
        kb, I = self.kb, self.ins
        self.ident_f = kb.sb([128, 128], F32); kb.dma("sync", self.ident_f[:], I["ident_f"][:, :])
        self.ident_b = kb.sb([128, 128], BF16); kb.dma("sync", self.ident_b[:], I["ident_b"][:, :])
        self.ones_f = kb.sb([128, 128], F32); kb.dma("sync", self.ones_f[:], I["ones_f"][:, :])
        self.ones_b = kb.sb([128, 128], BF16); kb.dma("sync", self.ones_b[:], I["ones_b"][:, :])
        self.eps_t = kb.sb([128, 1], F32); kb.memset(self.eps_t[:], EPS)
        self.xres = kb.dram("xres", [8, 128, T], F32)
        self.hT = kb.sb([128, 8, T], BF16, "hT")
        self.ybr = kb.dram("ybr", [4096, T], BF16)
        m = kb.mark()
        stg = [kb.sb([128, D], F32) for _ in range(2)]
        xo = [kb.sb([128, 8, 128], F32) for _ in range(2)]
        for n in range(NT):
            src = I["ctx"][n * 128:(n + 1) * 128, :] if n < 2 else I["x"][(n - 2) * 128:(n - 1) * 128, :]
            s = stg[n % 2]
            kb.dma("sync", s[:], src)
            for hb in range(2):
                ps = kb.ps()
                for j in range(4):
                    kt = hb * 4 + j
                    kb.tr(ps[:, j * 128:(j + 1) * 128], s[:, kt * 128:(kt + 1) * 128], self.ident_f[:])
                kb.cp(xo[n % 2][:, hb * 4:hb * 4 + 4, :],
                      ps[:].rearrange("p (j t) -> p j t", j=4), eng=("scalar" if hb else "vector"))
            kb.dma("sync", self.xres[:, :, n * 128:(n + 1) * 128].rearrange("k p t -> p k t"), xo[n % 2][:])
        kb.release(m)

    def load_rows_T(self, src2d, nrows, dst):
        kb = self.kb
        m = kb.mark()
        st = kb.sb([128, 128], F32)
        kb.dma("sync", st[0:nrows, :], src2d)
        ps = kb.ps()
        kb.tr(ps[:, 0:nrows], st[0:nrows, :], self.ident_f[0:nrows, 0:nrows])
        kb.cp(dst, ps[:, 0:nrows])
        kb.release(m)

    def modulation(self, l):
        kb, I = self.kb, self.ins
        self.mod = kb.sb([128, 48, 2], F32, "mod")
        self.sc1p = kb.sb([128, 8, 2], F32); self.sc2p = kb.sb([128, 8, 2], F32)
        m = kb.mark()
        c2 = kb.sb([2, D], F32)
        kb.dma("sync", c2[:], I["c2"][:, :])
        cs = kb.sb([2, D], F32)
        kb.act(cs[:], c2[:], AF.Silu)
        csT = kb.sb([128, 8, 2], F32)
        for kt in range(8):
            ps = kb.ps()
            kb.tr(ps[:, 0:2], cs[0:2, kt * 128:(kt + 1) * 128], self.ident_f[0:2, 0:2])
            kb.cp(csT[:, kt, :], ps[:, 0:2])
        ab = kb.sb([1, 6 * D], F32)
        kb.dma("sync", ab[:], I["ada_b"][l:l + 1, :])
        wb = [kb.sb([128, 8, 512], F32) for _ in range(2)]
        for cb in range(12):
            w = wb[cb % 2]
            kb.dma("sync", w[:], I["ada_w"][l, :, cb * 512:(cb + 1) * 512].rearrange("(k p) c -> p k c", p=128))
            ps = kb.ps()
            for j in range(4):
                o = ps[:, 2 * j:2 * j + 2]
                for kt in range(8):
                    kb.mm(o, w[:, kt, j * 128:(j + 1) * 128], csT[:, kt, :], start=(kt == 0), stop=False)
                jj = cb * 4 + j
                kb.mm(o, ab[0:1, jj * 128:(jj + 1) * 128], self.ones_f[0:1, 0:2], start=False, stop=True)
            kb.cp(self.mod[:, cb * 4:cb * 4 + 4, :], ps[:, 0:8].rearrange("p (j c) -> p j c", c=2))
        kb.ts(self.sc1p[:], self.mod[:, 8:16, :], 1.0, ALU.add)
        kb.ts(self.sc2p[:], self.mod[:, 32:40, :], 1.0, ALU.add)
        kb.release(m)
        self.sh1 = self.mod[:, 0:8, :]; self.gt1 = self.mod[:, 16:24, :]
        self.sh2 = self.mod[:, 24:32, :]; self.gt2 = self.mod[:, 40:48, :]

    def tok_chunks(self, t0, t1, step=512):
        out = []
        for a, b, col in ((0, LC, 1), (LC, T, 0)):
            a, b = max(a, t0), min(b, t1)
            while a < b:
                n = min(step, b - a)
                out.append((a, n, col))
                a += n
        return out

    def modulate(self, scp, sh, tok0=0):
        kb = self.kb
        m = kb.mark()
        xb = [kb.sb([128, 8, 512], F32) for _ in range(2)]
        for i, (t0, n, col) in enumerate(self.tok_chunks(tok0, T)):
            x = xb[i % 2]
            kb.dma("sync", x[:, :, 0:n], self.xres[:, :, t0:t0 + n].rearrange("k p t -> p k t"))
            for kt in range(8):
                kb.act(self.hT[:, kt, t0:t0 + n], x[:, kt, 0:n], AF.Identity, scale=scp[:, kt, col:col + 1], bias=sh[:, kt, col:col + 1])
        kb.release(m)

    def load_w(self, dst, src2d, q="gpsimd"):
        v = src2d.rearrange("(k p) c -> p k c", p=128)
        nk = v.shape[1]
        step = max(1, 1024 // 128)
        for k0 in range(0, nk, step):
            k1 = min(nk, k0 + step)
            self.kb.dma(q, dst[:, k0:k1, :], v[:, k0:k1, :])

    def proj_fm(self, w, j, tok0, ntok, nk=8, rhsT=None):
        kb = self.kb
        rhsT = self.hT if rhsT is None else rhsT
        t = tok0
        while t < tok0 + ntok:
            n = min(512, tok0 + ntok - t)
            ps = kb.ps()
            for kt in range(nk):
                kb.mm(ps[:, 0:n], w[:, kt, j * 128:(j + 1) * 128], rhsT[:, kt, t:t + n], start=(kt == 0), stop=(kt == nk - 1))
            yield ps, t, n
            t += n

    def retention(self, l, ctx_out):
        kb, I = self.kb, self.ins
        W = I["w_in"][l]
        m0 = kb.mark()
        rope = kb.sb([128, 2, T], F32, "rope")
        kb.dma("sync", rope[:], I["rope"][0:2].rearrange("f p t -> p f t"))
        rmask = kb.sb([128, 4, 128], F32); kb.dma("sync", rmask[:], I["rmask"].rearrange("h s t -> s h t"))
        rq = kb.sb([128, 8, 128], F32); kb.dma("sync", rq[:], I["rq"].rearrange("h d p t -> p (h d) t"))
        rk = kb.sb([128, 8], F32); kb.dma("sync", rk[:], I["rk"].rearrange("s h d -> s (h d)"))
        j8 = np.arange(8, dtype=np.float64)
        lg = np.log1p(-np.exp2(-5.0 - j8))
        sdec = [[float(np.exp(128.0 * lg[2 * h + d])) for d in range(2)] for h in range(4)]
        tok_out0 = 0 if ctx_out else LC
        tiles_out = list(range(0 if ctx_out else 2, NT))
        for hd in range(4):
            m1 = kb.mark()
            qrT = kb.sb([128, 2, T], BF16); krT = kb.sb([128, 2, T], BF16)
            vtm = kb.sb([128, NT, 512], BF16)
            kh = [kb.sb([128, NT, 256], BF16) for _ in range(2)]
            oT = kb.sb([128, 4, T], BF16)
            S = [[kb.sb([128, 512], F32) for _ in range(2)] for _ in range(2)]
            Sb = [[kb.sb([128, 512], BF16) for _ in range(2)] for _ in range(2)]
            scT = kb.sb([128, 128], BF16)
            qd = [kb.sb([128, 2, 128], BF16) for _ in range(2)]
            m2 = kb.mark()
            wq = kb.sb([128, 8, 256], BF16); self.load_w(wq[:], W[:, C_RQ + hd * 256:C_RQ + (hd + 1) * 256])
            wk = kb.sb([128, 8, 256], BF16); self.load_w(wk[:], W[:, C_RK + hd * 256:C_RK + (hd + 1) * 256])
            wv = kb.sb([128, 8, 512], BF16); self.load_w(wv[:], W[:, C_RV + hd * 512:C_RV + (hd + 1) * 512])
            raw = [kb.sb([128, 512], F32) for _ in range(2)]
            ta = kb.sb([128, 512], F32); tb = kb.sb([128, 512], F32)
            for (w, dst, sc) in ((wq, qrT, 1.0), (wk, krT, 1.0 / 16.0)):
                t0 = 0
                while t0 < T:
                    n = min(512, T - t0)
                    for j in range(2):
                        ps = kb.ps()
                        for kt in range(8):
                            kb.mm(ps[:, 0:n], w[:, kt, j * 128:(j + 1) * 128], self.hT[:, kt, t0:t0 + n], start=(kt == 0), stop=(kt == 7))
                        kb.act(raw[j][:, 0:n], ps[:, 0:n], AF.Copy, scale=sc)
                    cosT, sinT = rope[:, 0, t0:t0 + n], rope[:, 1, t0:t0 + n]
                    kb.tt(ta[:, 0:n], raw[0][:, 0:n], cosT, ALU.mult); kb.tt(tb[:, 0:n], raw[1][:, 0:n], sinT, ALU.mult)
                    kb.tt(dst[:, 0, t0:t0 + n], ta[:, 0:n], tb[:, 0:n], ALU.subtract)
                    kb.tt(ta[:, 0:n], raw[0][:, 0:n], sinT, ALU.mult); kb.tt(tb[:, 0:n], raw[1][:, 0:n], cosT, ALU.mult)
                    kb.tt(dst[:, 1, t0:t0 + n], ta[:, 0:n], tb[:, 0:n], ALU.add)
                    t0 += n
            for n in range(NT):
                ps = kb.ps()
                for kt in range(8):
                    kb.mm(ps[:], self.hT[:, kt, n * 128:(n + 1) * 128], wv[:, kt, :], start=(kt == 0), stop=(kt == 7))
                kb.cp(vtm[:, n, :], ps[:], eng="scalar")
                pt = kb.ps()
                ptb = pt[:].bitcast(BF16)
                for kt in range(2):
                    kb.tr(ptb[:, kt * 128:(kt + 1) * 128], krT[:, kt, n * 128:(n + 1) * 128], self.ident_b[:])
                for d in range(2):
                    kb.ts(kh[d][:, n, :], ptb[:, 0:256], rk[:, hd * 2 + d:hd * 2 + d + 1], ALU.mult)
            kb.release(m2)
            for d in range(2):
                for kt in range(2):
                    kb.memset(S[d][kt][:], 0.0); kb.memset(Sb[d][kt][:], 0.0)
            for d in range(2):
                order = list(range(NT)) if d == 0 else [1, 0] + list(range(NT - 1, 1, -1))
                for n in order:
                    tsl = slice(n * 128, (n + 1) * 128)
                    if n in tiles_out:
                        q_ = qd[n % 2]
                        kb.tt(q_[:], qrT[:, :, tsl], rq[:, hd * 2 + d, :].unsqueeze(1).broadcast_to([128, 2, 128]), ALU.mult)
                        po = kb.ps()
                        if d == 0:
                            psc = kb.ps()
                            for kt in range(2):
                                kb.mm(psc[:, 0:128], krT[:, kt, tsl], qrT[:, kt, tsl], start=(kt == 0), stop=(kt == 1))
                            kb.tt(scT[:], psc[:, 0:128], rmask[:, hd, :], ALU.mult)
                        for j in range(4):
                            o = po[:, j * 128:(j + 1) * 128]
                            if d == 0:
                                kb.mm(o, vtm[:, n, j * 128:(j + 1) * 128], scT[:], start=True, stop=False)
                            for kt in range(2):
                                kb.mm(o, Sb[d][kt][:, j * 128:(j + 1) * 128], q_[:, kt, :],
                                      start=(d == 1 and kt == 0), stop=(kt == 1))
                        ov = oT[:, :, tsl]
                        pv = po[:].rearrange("p (j t) -> p j t", j=4)
                        if d == 0:
                            kb.cp(ov, pv, eng="scalar")
                        else:
                            kb.tt(ov, ov, pv, ALU.add)
                    for kt in range(2):
                        pS = kb.ps()
                        kb.mm(pS[:], kh[d][:, n, kt * 128:(kt + 1) * 128], vtm[:, n, :])
                        kb.stt(S[d][kt][:], S[d][kt][:], sdec[hd][d], pS[:], ALU.mult, ALU.add)
                        kb.cp(Sb[d][kt][:], S[d][kt][:], eng="scalar")
            wg = kb.sb([128, 8, 512], BF16); self.load_w(wg[:], W[:, C_RG + hd * 512:C_RG + (hd + 1) * 512])
            sq = kb.sb([128, 4, 512], BF16); rstd = kb.sb([128, 512], F32); gs = kb.sb([128, 512], F32)
            ybs = [kb.sb([128, 4, 512], BF16) for _ in range(2)]
            r0 = 2048 + hd * 512
            t0 = tok_out0
            ci = 0
            while t0 < T:
                n = min(512, T - t0)
                yb = ybs[ci % 2]; ci += 1
                kb.act(sq[:, :, 0:n], oT[:, :, t0:t0 + n], AF.Square)
                pq = kb.ps()
                for j in range(4):
                    kb.mm(pq[:, 0:n], self.ones_b[:], sq[:, j, 0:n], start=(j == 0), stop=(j == 3))
                kb.act(rstd[:, 0:n], pq[:, 0:n], AF.Sqrt, scale=1.0 / 512.0, bias=self.eps_t[:])
                kb.op("vector", lambda e: e.reciprocal(out=rstd[:, 0:n], in_=rstd[:, 0:n]), [rstd[:]], [rstd[:]])
                for j in range(4):
                    pg = kb.ps()
                    for kt in range(8):
                        kb.mm(pg[:, 0:n], wg[:, kt, j * 128:(j + 1) * 128], self.hT[:, kt, t0:t0 + n], start=(kt == 0), stop=(kt == 7))
                    kb.act(gs[:, 0:n], pg[:, 0:n], AF.Silu)
                    kb.tt(gs[:, 0:n], gs[:, 0:n], rstd[:, 0:n], ALU.mult)
                    kb.tt(yb[:, j, 0:n], oT[:, j, t0:t0 + n], gs[:, 0:n], ALU.mult)
                kb.dma("sync", self.ybr[r0:r0 + 512, t0:t0 + n].rearrange("(j p) t -> p j t", p=128), yb[:, :, 0:n])
                t0 += n
            kb.release(m1)
        kb.release(m0)


    def hgrn2(self, l, ctx_out):
        kb, I = self.kb, self.ins
        W = I["w_in"][l]
        m0 = kb.mark()
        gmask = kb.sb([128, 2, 128], F32); kb.dma("sync", gmask[:], I["gmask"].rearrange("d s t -> s d t"))
        smask = kb.sb([128, T], F32); kb.dma("sync", smask[:], I["scanmask"][:, :])
        lbl = kb.sb([128, 32], F32)
        self.load_rows_T(I["hg_lb_logits"].rearrange("d l (k p) -> (d l k) p", p=128), 32, lbl[:])
        lb = kb.sb([128, 2, 8], F32); oml = kb.sb([128, 2, 8], F32)
        lv = lbl[:].rearrange("p (d l k) -> p d l k", d=2, l=2)
        if l == 0:
            kb.memset(lb[:], 0.0)
        else:
            kb.tt(lb[:], lv[:, :, 1, :], lv[:, :, 0, :], ALU.subtract)
            kb.act(lb[:], lb[:], AF.Sigmoid)
        kb.ts(oml[:], lb[:], -1.0, ALU.mult, 1.0, ALU.add)
        nw = kb.sb([128, 8], F32)
        self.load_rows_T(I["hg_norm_w"][l].rearrange("(k p) -> k p", p=128), 8, nw[:])
        tok_out0 = 0 if ctx_out else LC
        tiles_out = list(range(0 if ctx_out else 2, NT))
        NCH = T // 32
        for hd in range(8):
            m1 = kb.mark()
            qf = kb.sb([128, T], BF16); kf = kb.sb([128, T], BF16); qbi = kb.sb([128, T], BF16); kbk = kb.sb([128, T], BF16)
            qfo = kb.sb([128, T], F32); qbo = kb.sb([128, T], F32)
            khat = [kb.sb([128, NT, 128], BF16) for _ in range(2)]
            khz = [kb.sb([128, NT, 128], BF16) for _ in range(2)]
            vtm = kb.sb([128, NT, 128], BF16)
            etot = [kb.sb([128, NCH], F32) for _ in range(2)]
            oacc = kb.sb([128, T], F32)
            SS = [kb.sb([128, 128], F32) for _ in range(2)]
            scT = kb.sb([128, 128], BF16); scF = kb.sb([128, 128], F32)
            scT2 = kb.sb([128, 128], BF16); scF2 = kb.sb([128, 128], F32)
            SS2 = [kb.sb([128, 128], F32) for _ in range(2)]
            m2 = kb.mark()
            ws = {}
            for nm, c0 in (("ff", C_FF), ("fb", C_FB), ("q", C_HQ), ("v", C_HI)):
                ws[nm] = kb.sb([128, 8, 128], BF16)
                self.load_w(ws[nm][:], W[:, c0 + hd * 128:c0 + (hd + 1) * 128])
            tA = kb.sb([128, T], F32); tB = kb.sb([128, T], F32); tC = kb.sb([128, T], F32); tD = kb.sb([128, T], F32)
            tQ = kb.sb([128, T], F32); tK = kb.sb([128, T], BF16); tE = kb.sb([128, T], F32); totv = kb.sb([128, T // 32], F32)
            for ps, t0, n in self.proj_fm(ws["q"], 0, 0, T):
                kb.act(tQ[:, t0:t0 + n], ps[:, 0:n], AF.Silu)
            kb.ts(tQ[:], tQ[:], 128.0 ** -0.5, ALU.mult)
            for n in range(NT):
                ps = kb.ps()
                for kt in range(8):
                    kb.mm(ps[:, 0:128], self.hT[:, kt, n * 128:(n + 1) * 128], ws["v"][:, kt, :], start=(kt == 0), stop=(kt == 7))
                kb.cp(vtm[:, n, :], ps[:, 0:128], eng="scalar")
            for d in range(2):
                for ps, t0, n in self.proj_fm(ws["ff" if d == 0 else "fb"], 0, 0, T):
                    kb.act(tA[:, t0:t0 + n], ps[:, 0:n], AF.Sigmoid)
                kb.ts(tA[:], tA[:], oml[:, d, hd:hd + 1], ALU.mult, lb[:, d, hd:hd + 1], ALU.add)
                kb.act(tB[:], tA[:], AF.Ln)
                kb.ts(tA[:], tA[:], -1.0, ALU.mult, 1.0, ALU.add)
                kb.op("vector", lambda e: e.tensor_tensor_scan(out=tC[:], data0=smask[:], data1=tB[:], initial=0.0, op0=ALU.mult, op1=ALU.add),
                      [tC[:]], [smask[:], tB[:]])
                kb.act(etot[d][:], tC[:, 31::32], AF.Exp)
                kb.cp(totv[:], tC[:, 31::32])
                v3 = lambda a: a.rearrange("p (c k) -> p c k", k=32)
                bc = lambda a: a.unsqueeze(2).broadcast_to([128, NCH, 32])
                tb_ = bc(totv[:])
                if d == 0:
                    kb.tt(v3(tD[:]), v3(tC[:]), bc(tC[:, 15::32]), ALU.subtract)
                    kb.ts(tD[:], tD[:], 80.0, ALU.min, -80.0, ALU.max)
                    kb.act(tE[:], tD[:], AF.Exp)
                    kb.tt(qf[:], tQ[:], tE[:], ALU.mult)
                    kb.act(tE[:], tD[:], AF.Exp, scale=-1.0)
                    kb.tt(kf[:], tA[:], tE[:], ALU.mult)
                    kb.act(tE[:], tC[:], AF.Exp)
                    kb.tt(qfo[:], tQ[:], tE[:], ALU.mult)
                    kb.tt(v3(tD[:]), tb_, v3(tC[:]), ALU.subtract)
                    kb.act(tE[:], tD[:], AF.Exp)
                    kb.tt(tK[:], tA[:], tE[:], ALU.mult)
                else:
                    kb.tt(tC[:], tC[:], tB[:], ALU.subtract)
                    kb.tt(v3(tD[:]), v3(tC[:]), bc(tC[:, 16::32]), ALU.subtract)
                    kb.ts(tD[:], tD[:], 80.0, ALU.min, -80.0, ALU.max)
                    kb.act(tE[:], tD[:], AF.Exp)
                    kb.tt(kbk[:], tA[:], tE[:], ALU.mult)
                    kb.act(tE[:], tD[:], AF.Exp, scale=-1.0)
                    kb.tt(qbi[:], tQ[:], tE[:], ALU.mult)
                    kb.tt(v3(tD[:]), tb_, v3(tC[:]), ALU.subtract)
                    kb.act(tE[:], tD[:], AF.Exp)
                    kb.tt(qbo[:], tQ[:], tE[:], ALU.mult)
                    kb.act(tE[:], tC[:], AF.Exp)
                    kb.tt(tK[:], tA[:], tE[:], ALU.mult)
                for n in range(NT):
                    pt = kb.ps()
                    ptb = pt[:].bitcast(BF16)
                    kb.tr(ptb[:, 0:128], tK[:, n * 128:(n + 1) * 128], self.ident_b[:])
                    kb.cp(khat[d][:, n, :], ptb[:, 0:128], eng="scalar")
                kb.cp(khz[d][64:128, :, :], khat[d][64:128, :, :], eng="gpsimd")
                kb.memset(khz[d][64:96, :, :], 0.0, eng="gpsimd")
            kb.release(m2)
            SSd = [SS, SS2]
            scTd = [scT, scT2]; scFd = [scF, scF2]
            cur = [0, 0]
            for d in range(2):
                kb.memset(SSd[d][0][:], 0.0)
            orders = [list(range(NT)), [1, 0] + list(range(NT - 1, 1, -1))]
            QK = [(qf, kf, qfo), (qbi, kbk, qbo)]
            done = set()
            kb.ps_pool = [0, 1, 2, 3, 4, 5]
            for step in range(NT):
                ns = [orders[d][step] for d in range(2)]
                outs = [n in tiles_out for n in ns]
                pos = [kb.psum[6], kb.psum[7]]
                for d in range(2):
                    if outs[d]:
                        n = ns[d]
                        tsl = slice(n * 128, (n + 1) * 128)
                        psc = kb.ps()
                        kb.mm(psc[:, 0:128], QK[d][1][:, tsl], QK[d][0][:, tsl])
                        kb.ts(scFd[d][:], psc[:, 0:128], 1e30, ALU.min, -1e30, ALU.max)
                        kb.tt(scTd[d][:], scFd[d][:], gmask[:, d, :], ALU.mult, eng="gpsimd")
                        kb.mm(pos[d][:, 0:128], vtm[:, n, :], scTd[d][:], start=True, stop=False)
                for ci in range(4):
                    for d in range(2):
                        n = ns[d]
                        c = ci if d == 0 else 3 - ci
                        t32 = slice(n * 128 + c * 32, n * 128 + c * 32 + 32)
                        S_ = SSd[d]
                        if outs[d]:
                            kb.mm(pos[d][:, c * 32:c * 32 + 32], S_[cur[d]][:], QK[d][2][:, t32], start=False, stop=(ci == 3))
                        pS = kb.ps()
                        if c == 3:
                            kb.mm(pS[:, 0:128], khz[d][64:128, n, :], vtm[64:128, n, :])
                        else:
                            kb.mm(pS[:, 0:128], khat[d][c * 32:c * 32 + 32, n, :], vtm[c * 32:c * 32 + 32, n, :])
                        ch = n * 4 + c
                        kb.stt(S_[1 - cur[d]][:], S_[cur[d]][:], etot[d][:, ch:ch + 1], pS[:, 0:128], ALU.mult, ALU.add)
                        cur[d] = 1 - cur[d]
                for d in range(2):
                    if outs[d]:
                        n = ns[d]
                        tsl = slice(n * 128, (n + 1) * 128)
                        if n not in done:
                            kb.cp(oacc[:, tsl], pos[d][:, 0:128], eng="scalar")
                            done.add(n)
                        else:
                            kb.tt(oacc[:, tsl], oacc[:, tsl], pos[d][:, 0:128], ALU.add)
            kb.ps_pool = None
            wg = kb.sb([128, 8, 128], BF16); self.load_w(wg[:], W[:, C_HG + hd * 128:C_HG + (hd + 1) * 128])
            sq = kb.sb([128, 512], BF16); gs = kb.sb([128, 512], F32)
            ybs = [kb.sb([128, 512], BF16) for _ in range(2)]
            r0 = 1024 + hd * 128
            ci = 0
            for ps, t0, n in self.proj_fm(wg, 0, tok_out0, T - tok_out0):
                yb = ybs[ci % 2]; ci += 1
                kb.act(gs[:, 0:n], ps[:, 0:n], AF.Silu)
                kb.act(sq[:, 0:n], oacc[:, t0:t0 + n], AF.Square)
                pq = kb.ps()
                kb.mm(pq[:, 0:n], self.ones_b[:], sq[:, 0:n])
                if hd == 0:
                    kb.cp(self.ssq_hg[:, t0:t0 + n], pq[:, 0:n])
                else:
                    kb.tt(self.ssq_hg[:, t0:t0 + n], self.ssq_hg[:, t0:t0 + n], pq[:, 0:n], ALU.add)
                kb.stt(yb[:, 0:n], oacc[:, t0:t0 + n], nw[:, hd:hd + 1], gs[:, 0:n], ALU.mult, ALU.mult)
                kb.dma("sync", self.ybr[r0:r0 + 128, t0:t0 + n], yb[:, 0:n])
            kb.release(m1)
        kb.act(self.ssq_hg[:, tok_out0:T], self.ssq_hg[:, tok_out0:T], AF.Sqrt, scale=1.0 / 1024.0, bias=self.eps_t[:])
        kb.op("vector", lambda e: e.reciprocal(out=self.ssq_hg[:, tok_out0:T], in_=self.ssq_hg[:, tok_out0:T]), [self.ssq_hg[:]], [self.ssq_hg[:]])
        kb.release(m0)

    def hyena_filters(self, l, Ls, tag):
        kb, I = self.kb, self.ins
        nst, nblk = Ls // 128, (2 * Ls // 128) // 4
        N = 2 * Ls
        Kspec = kb.dram(f"kspec_{tag}_{l}", [2, 8, nblk, 128, 4, 256], F32)
        a_d = kb.dram(f"fa_{tag}_{l}", [Ls, 2048], BF16)
        b_d = kb.dram(f"fb_{tag}_{l}", [Ls, 2048], BF16)
        Fh = I[f"Fh{Ls}"]
        m0 = kb.mark()
        rn = kb.sb([128, 2048], F32)
        mA = kb.mark()
        w1 = kb.sb([33, 64], F32); kb.dma("sync", w1[:], I["hy_ff_w1"][l])
        w2 = kb.sb([64, 64], F32); kb.dma("sync", w2[:], I["hy_ff_w2"][l])
        w3 = kb.sb([64, 4096], F32); kb.dma("sync", w3[:], I["hy_ff_w3"][l])
        zT = kb.sb([33, Ls], F32); kb.dma("sync", zT[:], I[f"zT{Ls}"][:, :])
        st = kb.sb([4, 64], F32)
        kb.dma("sync", st[0:1, :], I["hy_ff_b1"][l:l + 1, :]); kb.dma("sync", st[1:2, :], I["hy_ff_b2"][l:l + 1, :])
        kb.dma("sync", st[2:4, :], I["hy_ff_freq"][l])
        pv = kb.sb([64, 4], F32)
        ps = kb.ps()
        kb.tr(ps[0:64, 0:4], st[0:4, :], self.ident_f[0:4, 0:4])
        kb.cp(pv[:], ps[0:64, 0:4])
        fb = kb.sb([64, 2], F32)
        kb.tt(fb[:, 0:1], pv[:, 0:1], pv[:, 2:3], ALU.mult); kb.tt(fb[:, 1:2], pv[:, 1:2], pv[:, 3:4], ALU.mult)
        hd1 = kb.sb([64, Ls], F32); hd2 = kb.sb([64, Ls], F32); wrapk = kb.sb([64, 512], F32)
        for (wm, kdim, src, dst, fi) in ((w1, 33, zT, hd1, 0), (w2, 64, hd1, hd2, 1)):
            t0 = 0
            while t0 < Ls:
                n = min(512, Ls - t0)
                ps = kb.ps()
                kb.mm(ps[0:64, 0:n], wm[0:kdim, :], src[0:kdim, t0:t0 + n])
                d_ = dst[:, t0:t0 + n]
                kb.act(d_, ps[0:64, 0:n], AF.Identity, scale=pv[:, 2 + fi:3 + fi], bias=fb[:, fi:fi + 1])
                k_ = wrapk[0:64, 0:n]
                kb.ts(k_, d_, 1.0 / TWO_PI, ALU.mult, 12582912.0, ALU.add)
                kb.ts(k_, k_, -12582912.0, ALU.add)
                kb.stt(d_, k_, -TWO_PI, d_, ALU.mult, ALU.add)
                kb.ts(d_, d_, math.pi, ALU.min, -math.pi, ALU.max)
                kb.act(d_, d_, AF.Sin)
                t0 += n
        kb.ps_pool = [0, 1, 2, 3]
        pn = [kb.psum[4 + i] for i in range(4)]
        hbuf = kb.sb([128, 8, 512], F32); habs = kb.sb([128, 8, 512], BF16)
        ab = [kb.sb([128, 2, 2048], BF16) for _ in range(2)]
        dec = [kb.sb([128, 1024], F32) for _ in range(2)]
        for tt_ in range(nst):
            dc = dec[tt_ % 2]
            kb.dma("sync", dc[:], I[f"dec{Ls}"][tt_ * 128:(tt_ + 1) * 128, :])
            for q in range(8):
                ps = kb.ps()
                kb.mm(ps[:], hd2[:, tt_ * 128:(tt_ + 1) * 128], w3[:, q * 512:(q + 1) * 512])
                kb.tt(hbuf[:, q, :], ps[:], dc[:, (q % 2) * 512:(q % 2) * 512 + 512], ALU.mult)
            if tt_ == 0:
                for q in (2, 3, 6, 7):
                    kb.memset(hbuf[0:1, q, :], 0.0)
            kb.act(habs[:], hbuf[:], AF.Abs)
            A_, B_ = ab[tt_ % 2][:, 0, :], ab[tt_ % 2][:, 1, :]
            for o in range(2):
                for hf in range(2):
                    csl = slice(o * 1024 + hf * 512, o * 1024 + hf * 512 + 512)
                    kb.tt(A_[:, csl], hbuf[:, o * 4 + hf, :], hbuf[:, o * 4 + 2 + hf, :], ALU.add, eng="gpsimd")
                    kb.tt(B_[:, csl], hbuf[:, o * 4 + hf, :], hbuf[:, o * 4 + 2 + hf, :], ALU.subtract, eng="gpsimd")
                    for dr in range(2):
                        kb.mm(pn[o * 2 + hf][:], self.ones_b[:], habs[:, o * 4 + dr * 2 + hf, :],
                              start=(tt_ == 0 and dr == 0), stop=(tt_ == nst - 1 and dr == 1))
            kb.dma("sync", a_d[tt_ * 128:(tt_ + 1) * 128, :], A_)
            kb.dma("sync", b_d[tt_ * 128:(tt_ + 1) * 128, :], B_)
        for i in range(4):
            kb.op("vector", lambda e: e.reciprocal(out=rn[:, i * 512:(i + 1) * 512], in_=pn[i][:]), [rn[:]], [pn[i][:]])
        kb.ts(rn[:], rn[:], 2.0 / N, ALU.mult)
        kb.ps_pool = None
        kb.release(mA)
        m1 = kb.mark()
        ach = kb.sb([128, nst, 512], BF16); bch = kb.sb([128, nst, 512], BF16)
        Fb = [kb.sb([128, nst, 512], BF16) for _ in range(2)]
        kq = [kb.sb([128, 4, 2, 512], F32) for _ in range(2)]
        for cc in range(4):
            o, ct0 = cc // 2, (cc % 2) * 4
            csl = slice(cc * 512, (cc + 1) * 512)
            av = a_d[:, csl].rearrange("(s p) c -> p s c", p=128); bv = b_d[:, csl].rearrange("(s p) c -> p s c", p=128)
            for k0 in range(0, nst, 8):
                k1 = min(nst, k0 + 8)
                kb.dma("sync", ach[:, k0:k1, :], av[:, k0:k1, :])
                kb.dma("sync", bch[:, k0:k1, :], bv[:, k0:k1, :])
            for blk in range(nblk):
                F_ = Fb[blk % 2]
                kb.dma("sync", F_[:], Fh[blk].rearrange("p (s f) -> p s f", f=512))
                K_ = kq[blk % 2]
                for i in range(4):
                    ps = kb.ps()
                    X = ach if i < 2 else bch
                    for s_ in range(nst):
                        kb.mm(ps[:], F_[:, s_, i * 128:(i + 1) * 128], X[:, s_, :], start=(s_ == 0), stop=(s_ == nst - 1))
                    if i < 2:
                        kb.tt(K_[:, 0, i, :], ps[:], rn[:, csl], ALU.mult)
                        kb.cp(K_[:, 3, i, :], K_[:, 0, i, :], eng="gpsimd")
                    else:
                        kb.tt(K_[:, 2, i - 2, :], ps[:], rn[:, csl], ALU.mult)
                        kb.ts(K_[:, 1, i - 2, :], K_[:, 2, i - 2, :], -1.0, ALU.mult, eng="gpsimd")
                if blk == 0:
                    ps = kb.ps()
                    for s_ in range(nst):
                        kb.mm(ps[0:1, :], F_[:, s_, 256:257], ach[:, s_, :], start=(s_ == 0), stop=(s_ == nst - 1))
                    kb.ts(K_[0:1, 0, 0, :], K_[0:1, 0, 0, :], 0.5, ALU.mult)
                    kb.memset(K_[0:1, 1, 0, :], 0.0); kb.memset(K_[0:1, 2, 0, :], 0.0)
                    kb.tt(K_[0:1, 3, 0, :], ps[0:1, :], rn[0:1, csl], ALU.mult)
                    kb.ts(K_[0:1, 3, 0, :], K_[0:1, 3, 0, :], 0.5, ALU.mult)
                for q in range(4):
                    for i in range(2):
                        kb.dma("sync", Kspec[o, ct0:ct0 + 4, blk, :, q, i * 128:(i + 1) * 128].rearrange("c p f -> p c f"),
                               K_[:, q, i, :].rearrange("p (c f) -> p c f", f=128))
        kb.release(m0)
        return Kspec

    def long_conv(self, z, o, ct, Ls, Kspec, bufs):
        kb, I = self.kb, self.ins
        nst, nblk = Ls // 128, (2 * Ls // 128) // 4
        Fh, Gh = I[f"Fh{Ls}"], I[f"Gh{Ls}"]
        zb, ztm, Yb, FG, Kq, U, tmp = bufs
        kb.cp(zb[:, 0:Ls], z, eng="gpsimd")
        for s0 in range(0, nst, 8):
            pt = kb.ps()
            ptb = pt[:].bitcast(BF16)
            k = min(8, nst - s0)
            for s_ in range(k):
                kb.tr(ptb[:, s_ * 128:(s_ + 1) * 128], zb[:, (s0 + s_) * 128:(s0 + s_ + 1) * 128], self.ident_b[:])
            kb.cp(ztm[:, s0:s0 + k, :], ptb[:, 0:k * 128].rearrange("p (s c) -> p s c", c=128), eng="scalar")
        for blk in range(nblk):
            F_ = FG[blk % 2]
            kb.dma("sync", F_[:, 0:nst * 512], Fh[blk])
            K_ = Kq[blk % 2]
            kb.dma("sync", K_[:], Kspec[o, ct, blk])
            pu = kb.ps()
            Fv = F_[:, 0:nst * 512].rearrange("p (s f) -> p s f", f=512)
            for i in range(4):
                for s_ in range(nst):
                    kb.mm(pu[:, i * 128:(i + 1) * 128], Fv[:, s_, i * 128:(i + 1) * 128], ztm[:, s_, :], start=(s_ == 0), stop=(s_ == nst - 1))
            kb.cp(U[:], pu[:], eng="scalar")
            Uc, Us = U[:, 0:256], U[:, 256:512]
            kb.tt(tmp[:, 0, :], Uc, K_[:, 0, :], ALU.mult); kb.tt(tmp[:, 1, :], Us, K_[:, 1, :], ALU.mult)
            kb.tt(Yb[:, blk, 0, :], tmp[:, 0, :], tmp[:, 1, :], ALU.add, eng="gpsimd")
            kb.tt(tmp[:, 2, :], Uc, K_[:, 2, :], ALU.mult); kb.tt(tmp[:, 3, :], Us, K_[:, 3, :], ALU.mult)
            kb.tt(Yb[:, blk, 1, :], tmp[:, 2, :], tmp[:, 3, :], ALU.add, eng="gpsimd")
        ntc = max(1, Ls // 512)
        py = [kb.psum[4 + i] for i in range(ntc)]
        for blk in range(nblk):
            G_ = FG[blk % 2]
            kb.dma("sync", G_[:, 0:4 * Ls], Gh[blk])
            Gv = G_[:, 0:4 * Ls].rearrange("p (i t) -> p i t", i=4)
            for i in range(4):
                lhs = Yb[:, blk, i // 2, (i % 2) * 128:(i % 2) * 128 + 128]
                for tc in range(ntc):
                    n = min(512, Ls)
                    kb.mm(py[tc][:, 0:n], lhs, Gv[:, i, tc * 512:tc * 512 + n], start=(blk == 0 and i == 0), stop=(blk == nblk - 1 and i == 3))
        return py

    def hyena(self, l, ctx_out, Ksp_lat, Ksp_ctx):
        kb, I = self.kb, self.ins
        W = I["w_in"][l]
        m0 = kb.mark()
        cwr = kb.sb([9, 3072], F32); kb.dma("sync", cwr[:], I["hy_conv_w"][l])
        cw = kb.sb([128, 24, 9], F32)
        for g in range(24):
            ps = kb.ps()
            kb.tr(ps[:, 0:9], cwr[0:9, g * 128:(g + 1) * 128], self.ident_f[0:9, 0:9])
            kb.cp(cw[:, g, :], ps[:, 0:9])
        cb = kb.sb([128, 24], F32)
        self.load_rows_T(I["hy_conv_b"][l].rearrange("(k p) -> k p", p=128), 24, cb[:])
        skp = kb.sb([128, 16], F32)
        self.load_rows_T(I["hy_skip"][l].rearrange("o (k p) -> (o k) p", p=128), 16, skp[:])
        upad = kb.sb([128, 34, 66], BF16); kb.memset(upad[:], 0.0)
        cpad = kb.sb([128, 258], BF16); kb.memset(cpad[:], 0.0)
        dg = kb.sb([128, 9, 128], BF16)
        zc = [kb.sb([128, T], F32) for _ in range(3)]
        z1 = kb.sb([128, T], F32)
        wb = [kb.sb([128, 8, 128], BF16) for _ in range(2)]
        zb = kb.sb([128, L], BF16); ztm = kb.sb([128, 16, 128], BF16); Yb = kb.sb([128, 8, 2, 256], BF16)
        FG = [kb.sb([128, 8192], BF16) for _ in range(2)]
        Kq = [kb.sb([128, 4, 256], F32) for _ in range(2)]
        U = kb.sb([128, 512], F32); tmp = kb.sb([128, 4, 256], F32)
        bufs = (zb, ztm, Yb, FG, Kq, U, tmp)
        yo = [kb.sb([128, 512], BF16) for _ in range(2)]
        kb.ps_pool = [0, 1, 2, 3]
        for ct in range(8):
            for g in range(3):
                gi = g * 8 + ct
                w = wb[g % 2]
                self.load_w(w[:], W[:, C_HY + gi * 128:C_HY + (gi + 1) * 128])
                for tap in range(9):
                    kb.ts(dg[:, tap, :], self.ident_b[:], cw[:, gi, tap:tap + 1], ALU.mult)
                if ctx_out:
                    for ps, t0, n in self.proj_fm(w, 0, 0, LC):
                        kb.cp(cpad[:, 1:257], ps[:, 0:256], eng="scalar")
                    ps2 = kb.ps()
                    for j in range(3):
                        kb.mm(ps2[:, 0:256], dg[:, 3 + j, :], cpad[:, j:j + 256], start=(j == 0), stop=(j == 2))
                    kb.act(zc[g][:, 0:LC], ps2[:, 0:256], AF.Identity, bias=cb[:, gi:gi + 1])
                ci = 0
                for ps, t0, n in self.proj_fm(w, 0, LC, L):
                    kb.cp(upad[:, 1 + 8 * ci:9 + 8 * ci, 1:65], ps[:].rearrange("p (r c) -> p r c", c=64), eng="scalar")
                    ci += 1
                for ci in range(4):
                    ps2 = kb.ps()
                    for tap in range(9):
                        dr, dc = tap // 3, tap % 3
                        kb.mm(ps2[:], dg[:, tap, :], upad[:, 8 * ci + dr:8 * ci + dr + 8, dc:dc + 64], start=(tap == 0), stop=(tap == 8))
                    kb.act(zc[g][:, LC + ci * 512:LC + (ci + 1) * 512], ps2[:], AF.Identity, bias=cb[:, gi:gi + 1])
            for (t0, Ls, Ksp) in (((0, LC, Ksp_ctx),) if ctx_out else ()) + ((LC, L, Ksp_lat),):
                zin = zc[0]
                for o in range(2):
                    py = self.long_conv(zin[:, t0:t0 + Ls], o, ct, Ls, Ksp, bufs)
                    gate = zc[1 + o]
                    for tc, p_ in enumerate(py):
                        n = min(512, Ls)
                        sl = slice(t0 + tc * 512, t0 + tc * 512 + n)
                        kb.stt(z1[:, sl], zin[:, sl], skp[:, o * 8 + ct:o * 8 + ct + 1], p_[:, 0:n], ALU.mult, ALU.add)
                        if o == 0:
                            kb.tt(z1[:, sl], z1[:, sl], gate[:, sl], ALU.mult)
                        else:
                            y_ = yo[tc % 2]
                            kb.tt(y_[:, 0:n], z1[:, sl], gate[:, sl], ALU.mult)
                            kb.dma("sync", self.ybr[ct * 128:(ct + 1) * 128, sl], y_[:, 0:n])
                    zin = z1
        kb.ps_pool = None
        kb.release(m0)

    def ln_stats(self, tp, n, bufs):
        kb = self.kb
        sqb, mean, rstd = bufs
        pm = kb.ps(); pq = kb.ps()
        for mt in range(8):
            kb.mm(pm[:, 0:n], self.ones_f[:], tp[:, mt, 0:n], start=(mt == 0), stop=(mt == 7))
        for mt in range(8):
            kb.act(sqb[:, 0:n], tp[:, mt, 0:n], AF.Square)
            kb.mm(pq[:, 0:n], self.ones_f[:], sqb[:, 0:n], start=(mt == 0), stop=(mt == 7))
        kb.ts(mean[:, 0:n], pm[:, 0:n], 1.0 / D, ALU.mult)
        kb.tt(rstd[:, 0:n], mean[:, 0:n], mean[:, 0:n], ALU.mult)
        kb.stt(rstd[:, 0:n], pq[:, 0:n], 1.0 / D, rstd[:, 0:n], ALU.mult, ALU.subtract)
        kb.act(rstd[:, 0:n], rstd[:, 0:n], AF.Sqrt, bias=self.eps_t[:])
        kb.op("vector", lambda e: e.reciprocal(out=rstd[:, 0:n], in_=rstd[:, 0:n]), [rstd[:]], [rstd[:]])
        return mean, rstd

    def ln_apply(self, tp, n, mean, rstd, lng, lnb):
        kb = self.kb
        for mt in range(8):
            kb.tt(tp[:, mt, 0:n], tp[:, mt, 0:n], mean[:, 0:n], ALU.subtract)
            kb.tt(tp[:, mt, 0:n], tp[:, mt, 0:n], rstd[:, 0:n], ALU.mult)
            kb.ts(tp[:, mt, 0:n], tp[:, mt, 0:n], lng[:, mt:mt + 1], ALU.mult, lnb[:, mt:mt + 1], ALU.add)

    def load_ln(self, l, which):
        kb, I = self.kb, self.ins
        g = kb.sb([128, 8], F32); b = kb.sb([128, 8], F32)
        self.load_rows_T(I[f"ln{which}_g"][l].rearrange("(k p) -> k p", p=128), 8, g[:])
        self.load_rows_T(I[f"ln{which}_b"][l].rearrange("(k p) -> k p", p=128), 8, b[:])
        return g, b

    def merge(self, l, ctx_out, moe):
        kb, I = self.kb, self.ins
        W = I["w_in"][l]
        m0 = kb.mark()
        lng, lnb = self.load_ln(l, 1)
        ych = kb.sb([128, 32, 512], BF16)
        macc = kb.sb([128, 8, 512], F32); mT = kb.sb([128, 8, 512], BF16)
        xc = kb.sb([128, 8, 512], F32); tp = kb.sb([128, 8, 512], F32)
        g_ = kb.sb([128, 512], F32); t_ = kb.sb([128, 512], F32)
        sqb = kb.sb([128, 512], F32); mean = kb.sb([128, 512], F32); rstd = kb.sb([128, 512], F32)
        wp = [kb.sb([128, 16, 512], BF16) for _ in range(2)]
        wg = [kb.sb([128, 8, 512], BF16) for _ in range(2)]
        if moe:
            rt = kb.sb([128, 8, 8], F32)
            kb.dma("sync", rt[:], I["moe_router"][0].rearrange("(k p) e -> p k e", p=128))
            lg = kb.sb([128, 8], F32); mx = kb.sb([128, 8], F32); g12 = kb.sb([128, 2], F32); wa = kb.sb([128, 8], F32); wb2 = kb.sb([128, 8], F32)
        P_ = (I["p_hy"][l], I["p_hg"][l], I["p_rt"][l])
        K0 = (0, 8, 16); NK = (8, 8, 16)
        wi = 0
        for (t0, n, col) in self.tok_chunks(0 if ctx_out else LC, T):
            yv = self.ybr[:, t0:t0 + n].rearrange("(k p) t -> p k t", p=128)
            for k0 in range(0, 32, 8):
                kb.dma("sync", ych[:, k0:k0 + 8, 0:n], yv[:, k0:k0 + 8, :])
            kb.dma("sync", xc[:, :, 0:n], self.xres[:, :, t0:t0 + n].rearrange("k p t -> p k t"))
            items = [(b, mh) for b in range(3) for mh in range(2)]

            def load_item(idx, slot):
                b_, mh_ = items[idx]
                self.load_w(wp[slot % 2][:, 0:NK[b_], :], P_[b_][:, mh_ * 512:(mh_ + 1) * 512])
                self.load_w(wg[slot % 2][:], W[:, C_BR + b_ * 1024 + mh_ * 512:C_BR + b_ * 1024 + (mh_ + 1) * 512])
            load_item(0, wi)
            for ii, (b, mh) in enumerate(items):
                w = wp[wi % 2]; w2 = wg[wi % 2]; wi += 1
                if ii + 1 < len(items):
                    load_item(ii + 1, wi)
                for m4 in range(4):
                    mt = mh * 4 + m4
                    msl = slice(m4 * 128, (m4 + 1) * 128)
                    ps = kb.ps()
                    for kt in range(NK[b]):
                        kb.mm(ps[:, 0:n], w[:, kt, msl], ych[:, K0[b] + kt, 0:n], start=(kt == 0), stop=(kt == NK[b] - 1))
                    pg = kb.ps()
                    for kt in range(8):
                        kb.mm(pg[:, 0:n], w2[:, kt, msl], self.hT[:, kt, t0:t0 + n], start=(kt == 0), stop=(kt == 7))
                    kb.act(g_[:, 0:n], pg[:, 0:n], AF.Sigmoid)
                    if b == 1:
                        kb.tt(g_[:, 0:n], g_[:, 0:n], self.ssq_hg[:, t0:t0 + n], ALU.mult)
                    if b == 0:
                        kb.tt(macc[:, mt, 0:n], ps[:, 0:n], g_[:, 0:n], ALU.mult)
                    else:
                        kb.tt(t_[:, 0:n], ps[:, 0:n], g_[:, 0:n], ALU.mult)
                        kb.tt(macc[:, mt, 0:n], macc[:, mt, 0:n], t_[:, 0:n], ALU.add)
            kb.cp(mT[:, :, 0:n], macc[:, :, 0:n], eng="scalar")
            self.load_w(wg[wi % 2][:], I["w_o"][l][:, 0:512])
            for mh in range(2):
                w = wg[wi % 2]; wi += 1
                if mh == 0:
                    self.load_w(wg[wi % 2][:], I["w_o"][l][:, 512:1024])
                for m4 in range(4):
                    mt = mh * 4 + m4
                    ps = kb.ps()
                    for kt in range(8):
                        kb.mm(ps[:, 0:n], w[:, kt, m4 * 128:(m4 + 1) * 128], mT[:, kt, 0:n], start=(kt == 0), stop=(kt == 7))
                    kb.act(xc[:, mt, 0:n], xc[:, mt, 0:n], AF.Copy, scale=ALPHA)
                    kb.stt(tp[:, mt, 0:n], ps[:, 0:n], self.gt1[:, mt, col:col + 1], xc[:, mt, 0:n], ALU.mult, ALU.add)
            mean_, rstd_ = self.ln_stats(tp, n, (sqb, mean, rstd))
            self.ln_apply(tp, n, mean_, rstd_, lng, lnb)
            kb.dma("sync", self.xres[:, :, t0:t0 + n].rearrange("k p t -> p k t"), tp[:, :, 0:n])
            for kt in range(8):
                kb.act(xc[:, kt, 0:n], tp[:, kt, 0:n], AF.Identity, scale=self.sc2p[:, kt, col:col + 1], bias=self.sh2[:, kt, col:col + 1])
            kb.cp(self.hT[:, :, t0:t0 + n], xc[:, :, 0:n], eng="scalar")
            if moe:
                for tt_ in range(n // 128):
                    pl = kb.ps()
                    for kt in range(8):
                        kb.mm(pl[:, 0:8], xc[:, kt, tt_ * 128:(tt_ + 1) * 128], rt[:, kt, :], start=(kt == 0), stop=(kt == 7))
                    kb.cp(lg[:], pl[:, 0:8])
                    kb.op("vector", lambda e: e.max(out=mx[:], in_=lg[:]), [mx[:]], [lg[:]])
                    kb.tt(g12[:, 0:1], mx[:, 0:1], mx[:, 1:2], ALU.subtract)
                    kb.act(g12[:, 1:2], g12[:, 0:1], AF.Sigmoid, scale=-1.0)
                    kb.act(g12[:, 0:1], g12[:, 0:1], AF.Sigmoid)
                    kb.ts(wa[:], lg[:], mx[:, 0:1], ALU.is_equal, g12[:, 0:1], ALU.mult)
                    kb.ts(wb2[:], lg[:], mx[:, 1:2], ALU.is_equal, g12[:, 1:2], ALU.mult)
                    kb.tt(wa[:], wa[:], wb2[:], ALU.add)
                    pt = kb.ps()
                    kb.tr(pt[0:8, 0:128], wa[:], self.ident_f[:])
                    kb.cp(self.gateT[0:8, t0 + tt_ * 128:t0 + (tt_ + 1) * 128], pt[0:8, 0:128])
        kb.release(m0)

    def ffn_dense(self, l):
        kb, I = self.kb, self.ins
        W1, W3, W2 = I["ffn_w1"][0], I["ffn_w3"][0], I["ffn_w2"][0]
        m0 = kb.mark()
        lng, lnb = self.load_ln(l, 2)
        hid = kb.sb([128, 22, 512], BF16)
        w1b = [kb.sb([128, 8, 512], BF16) for _ in range(2)]; w3b = [kb.sb([128, 8, 512], BF16) for _ in range(2)]
        w2t = [kb.sb([128, 22, 512], BF16) for _ in range(2)]
        xc = kb.sb([128, 8, 512], F32); tp = kb.sb([128, 8, 512], F32)
        s_ = [kb.sb([128, 512], F32) for _ in range(2)]
        sqb = kb.sb([128, 512], F32); mean = kb.sb([128, 512], F32); rstd = kb.sb([128, 512], F32)
        for (t0, n, col) in self.tok_chunks(0, T):
            kb.dma("sync", xc[:, :, 0:n], self.xres[:, :, t0:t0 + n].rearrange("k p t -> p k t"))
            def load_fblk(jb):
                nj_ = min(4, 22 - jb * 4)
                self.load_w(w1b[jb % 2][:, :, 0:nj_ * 128], W1[:, jb * 512:jb * 512 + nj_ * 128])
                self.load_w(w3b[jb % 2][:, :, 0:nj_ * 128], W3[:, jb * 512:jb * 512 + nj_ * 128])
            load_fblk(0)
            for jb in range(6):
                nj = min(4, 22 - jb * 4)
                a, b = w1b[jb % 2], w3b[jb % 2]
                if jb + 1 < 6:
                    load_fblk(jb + 1)
                else:
                    self.load_w(w2t[0][:], W2[:, 0:512])
                for j in range(nj):
                    p1 = kb.ps(); p3 = kb.ps()
                    for kt in range(8):
                        kb.mm(p1[:, 0:n], a[:, kt, j * 128:(j + 1) * 128], self.hT[:, kt, t0:t0 + n], start=(kt == 0), stop=(kt == 7))
                    for kt in range(8):
                        kb.mm(p3[:, 0:n], b[:, kt, j * 128:(j + 1) * 128], self.hT[:, kt, t0:t0 + n], start=(kt == 0), stop=(kt == 7))
                    sj = s_[j % 2]
                    kb.act(sj[:, 0:n], p1[:, 0:n], AF.Silu)
                    kb.tt(hid[:, jb * 4 + j, 0:n], sj[:, 0:n], p3[:, 0:n], ALU.mult)
            for mt in range(8):
                w = w2t[mt // 4]
                if mt == 0:
                    self.load_w(w2t[1][:], W2[:, 512:1024])
                ps = kb.ps()
                for j in range(22):
                    kb.mm(ps[:, 0:n], w[:, j, (mt % 4) * 128:(mt % 4 + 1) * 128], hid[:, j, 0:n], start=(j == 0), stop=(j == 21))
                kb.act(xc[:, mt, 0:n], xc[:, mt, 0:n], AF.Copy, scale=ALPHA)
                kb.stt(tp[:, mt, 0:n], ps[:, 0:n], self.gt2[:, mt, col:col + 1], xc[:, mt, 0:n], ALU.mult, ALU.add)
            mean_, rstd_ = self.ln_stats(tp, n, (sqb, mean, rstd))
            self.ln_apply(tp, n, mean_, rstd_, lng, lnb)
            kb.dma("sync", self.xres[:, :, t0:t0 + n].rearrange("k p t -> p k t"), tp[:, :, 0:n])
        kb.release(m0)

    def moe(self, l):
        kb, I = self.kb, self.ins
        m0 = kb.mark()
        lng, lnb = self.load_ln(l, 2)
        sel = kb.sb([8, 1024], F32); kb.dma("sync", sel[:], I["sel8"][:, :])
        hid = kb.sb([128, 28, 512], BF16)
        facc = kb.sb([128, 8, 512], F32)
        w1b = [kb.sb([128, 8, 512], BF16) for _ in range(2)]; w3b = [kb.sb([128, 8, 512], BF16) for _ in range(2)]
        w2h = [kb.sb([128, 28, 512], BF16)] * 2
        xc = kb.sb([128, 8, 512], F32)
        gb = kb.sb([128, 512], F32)
        s_ = [kb.sb([128, 512], F32) for _ in range(2)]
        sqb = kb.sb([128, 512], F32); mean = kb.sb([128, 512], F32); rstd = kb.sb([128, 512], F32)
        otm = [kb.sb([128, D], F32) for _ in range(2)]
        WE = [(I["moe_w1"][0, e], I["moe_w3"][0, e], I["moe_w2"][0, e]) for e in range(NEXP)]

        def load_blk(e, jb):
            self.load_w(w1b[jb % 2][:], WE[e][0][:, jb * 512:(jb + 1) * 512])
            self.load_w(w3b[jb % 2][:], WE[e][1][:, jb * 512:(jb + 1) * 512])

        chunks = self.tok_chunks(LC, T)
        for ci_, (t0, n, col) in enumerate(chunks):
            kb.dma("sync", xc[:, :, 0:n], self.xres[:, :, t0:t0 + n].rearrange("k p t -> p k t"))
            if ci_ == 0:
                load_blk(0, 0)
            for e in range(NEXP):
                W1, W3, W2 = WE[e]
                pb = kb.ps()
                kb.mm(pb[:, 0:n], sel[0:8, e * 128:(e + 1) * 128], self.gateT[0:8, t0:t0 + n])
                kb.cp(gb[:, 0:n], pb[:, 0:n], eng="scalar")
                w = w2h[0]
                self.load_w(w[:], W2[:, 0:512])
                for jb in range(7):
                    if jb + 1 < 7:
                        load_blk(e, jb + 1)
                    a, b = w1b[jb % 2], w3b[jb % 2]
                    for j in range(4):
                        p1 = kb.ps(); p3 = kb.ps()
                        for kt in range(8):
                            kb.mm(p1[:, 0:n], a[:, kt, j * 128:(j + 1) * 128], self.hT[:, kt, t0:t0 + n], start=(kt == 0), stop=(kt == 7))
                        for kt in range(8):
                            kb.mm(p3[:, 0:n], b[:, kt, j * 128:(j + 1) * 128], self.hT[:, kt, t0:t0 + n], start=(kt == 0), stop=(kt == 7))
                        sj = s_[j % 2]
                        kb.act(sj[:, 0:n], p1[:, 0:n], AF.Silu)
                        kb.tt(hid[:, jb * 4 + j, 0:n], sj[:, 0:n], p3[:, 0:n], ALU.mult)
                if e + 1 < NEXP:
                    load_blk(e + 1, 0)
                elif ci_ + 1 < len(chunks):
                    load_blk(0, 0)
                for hf in range(2):
                    if hf == 1:
                        self.load_w(w[:], W2[:, 512:1024])
                    for m4 in range(4):
                        mt = hf * 4 + m4
                        ps = kb.ps()
                        for j in range(28):
                            kb.mm(ps[:, 0:n], w[:, j, m4 * 128:(m4 + 1) * 128], hid[:, j, 0:n], start=(j == 0), stop=(j == 27))
                        if e == 0:
                            kb.tt(facc[:, mt, 0:n], ps[:, 0:n], gb[:, 0:n], ALU.mult)
                        else:
                            sj = s_[m4 % 2]
                            kb.tt(sj[:, 0:n], ps[:, 0:n], gb[:, 0:n], ALU.mult)
                            kb.tt(facc[:, mt, 0:n], facc[:, mt, 0:n], sj[:, 0:n], ALU.add)
            for mt in range(8):
                kb.act(xc[:, mt, 0:n], xc[:, mt, 0:n], AF.Copy, scale=ALPHA)
                kb.stt(facc[:, mt, 0:n], facc[:, mt, 0:n], self.gt2[:, mt, col:col + 1], xc[:, mt, 0:n], ALU.mult, ALU.add)
            mean_, rstd_ = self.ln_stats(facc, n, (sqb, mean, rstd))
            self.ln_apply(facc, n, mean_, rstd_, lng, lnb)
            for tt_ in range(n // 128):
                o_ = otm[tt_ % 2]
                for hb in range(2):
                    ps = kb.ps()
                    for j in range(4):
                        kb.tr(ps[:, j * 128:(j + 1) * 128], facc[:, hb * 4 + j, tt_ * 128:(tt_ + 1) * 128], self.ident_f[:])
                    kb.cp(o_[:, hb * 512:(hb + 1) * 512], ps[:], eng=("scalar" if hb else "vector"))
                r0 = t0 - LC + tt_ * 128
                kb.dma("sync", self.out[r0:r0 + 128, :], o_[:])
        kb.release(m0)

    def build_all(self, nlayers=2, stages=("ret", "hg", "hy", "merge", "ffn")):
        kb = self.kb
        self.setup()
        for l in range(nlayers):
            ctx_out = (l == 0)
            m = kb.mark()
            self.ssq_hg = kb.sb([128, T], F32, "ssq_hg")
            self.gateT = kb.sb([8, T], F32, "gateT")
            self.modulation(l)
            self.modulate(self.sc1p, self.sh1)
            Kl = self.hyena_filters(l, 2048, "lat") if "hy" in stages else None
            Kc = self.hyena_filters(l, 256, "ctx") if (ctx_out and "hy" in stages) else None
            if "ret" in stages:
                self.retention(l, ctx_out)
            if "hg" in stages:
                self.hgrn2(l, ctx_out)
            if "hy" in stages:
                self.hyena(l, ctx_out, Kl, Kc)
            if "merge" in stages:
                self.merge(l, ctx_out, moe=(l == 1))
            if "ffn" in stages:
                if l == 0:
                    self.ffn_dense(l)
                else:
                    self.moe(l)
            kb.release(m)
        self.finish()

    def finish(self, final_src=None):
        kb = self.kb
        kb.barrier()
        kb.finish()


def prep_inputs(inputs, consts):
    shared = {}
    for n, shp in W_SHAPES.items():
        shared[n] = np.ascontiguousarray(np.asarray(inputs[n], dtype=np.float32).reshape(shp))
    shared.update(consts)
    maps = []
    for b in range(8):
        m = dict(shared)
        m["x"] = np.ascontiguousarray(inputs["x"][b], dtype=np.float32)
        m["ctx"] = np.ascontiguousarray(inputs["ctx"][b], dtype=np.float32)
        m["c2"] = np.ascontiguousarray(np.stack([inputs["c"][b], inputs["c_ctx"]]), dtype=np.float32)
        maps.append(m)
    return maps


_CACHE = {}


def kernel(**inputs):
    if "c" not in _CACHE:
        _CACHE["c"] = make_consts()
    consts = _CACHE["c"]
    P = Prog(consts)
    P.build_all()
    maps = prep_inputs(inputs, consts)
    res = run_bass_kernel_spmd(P.nc, maps, core_ids=list(range(8)))
    return np.stack([np.asarray(r["out"], dtype=np.float32) for r in res.results], axis=0)
```

```python
import math
import numpy as np
import ml_dtypes
import concourse.bass as bass
import concourse.mybir as mybir
from concourse.bass_utils import run_bass_kernel_spmd

F32 = mybir.dt.float32
BF16 = mybir.dt.bfloat16
AF = mybir.ActivationFunctionType
ALU = mybir.AluOpType

D = 1024
L = 2048
LC = 256
T = L + LC
NT = T // 128
DEPTH = 2
NCOL = 17408
D_FF = 2816
NEXP = 8
D_EXP = 3584
EPS = 1e-5
ALPHA = (2 * DEPTH) ** 0.25
C_FF, C_FB, C_HI, C_RK, C_RV = 0, 1024, 2048, 3072, 4096
C_HQ, C_HG, C_RQ, C_RG, C_HY, C_BR = 6144, 7168, 8192, 9216, 11264, 14336
TWO_PI = 2.0 * math.pi


class Res:
    __slots__ = ("w", "rs")

    def __init__(self):
        self.w = None
        self.rs = {}


class Eng:
    def __init__(self, nc, name, eng):
        self.name = name
        self.eng = eng
        self.sem = nc.alloc_semaphore(name="sem_" + name)
        self.cnt = 0
        self.waited = {}


class KB:
    def __init__(self, nc):
        self.nc = nc
        self.E = {n: Eng(nc, n, getattr(nc, n)) for n in ("tensor", "vector", "scalar", "gpsimd", "sync")}
        self.res = {}
        self.slots = {}
        for q in ("sync", "gpsimd"):
            self.slots[q] = [[nc.alloc_semaphore(name=f"dq_{q}{i}"), 0] for i in range(12)]
        self.slot_i = {"sync": 0, "gpsimd": 0}
        self.n_sb = 0
        self.sb_off = (nc.sbuf_base + 63) // 64 * 64
        self.sb_top = nc.sbuf_top
        self.psum = [nc.alloc_psum_tensor(f"ps{i}", [128, 512], F32) for i in range(8)]
        self.ps_i = 0

    def sb(self, shape, dt, name=None):
        self.n_sb += 1
        nbytes = int(np.prod(shape[1:])) * (2 if dt == BF16 else 4)
        nbytes = (nbytes + 63) // 64 * 64
        off = self.sb_off
        assert off + nbytes <= self.sb_top, f"SBUF overflow {off}+{nbytes} > {self.sb_top}"
        self.sb_off += nbytes
        return self.nc.alloc_sbuf_tensor_at((name or "sb") + f"_{self.n_sb}", list(shape), dt, offset=off)

    def mark(self):
        return self.sb_off

    def release(self, m):
        self.barrier()
        self.sb_off = m

    def barrier(self):
        tks = [(e.sem, e.cnt) for e in self.E.values() if e.cnt > 0]
        for q in self.slots:
            tks += [(sl[0], sl[1]) for sl in self.slots[q] if sl[1] > 0]
        for e in self.E.values():
            for tk in tks:
                self._wait(e, tk)

    def dram(self, name, shape, dt):
        kind = "ExternalOutput" if name in getattr(self, "dbg", ()) else "Internal"
        return self.nc.dram_tensor(name, list(shape), dt, kind=kind).ap()

    def ps(self):
        pool = getattr(self, "ps_pool", None) or list(range(8))
        p = self.psum[pool[self.ps_i % len(pool)]]
        self.ps_i += 1
        return p

    def _r(self, ap):
        if isinstance(ap, Res):
            return ap
        n = ap.tensor.name
        r = self.res.get(n)
        if r is None:
            r = self.res[n] = Res()
        return r

    def _wait(self, E, tk):
        if tk is None:
            return
        sem, val = tk
        if E.name == "tensor" and sem is E.sem:
            return
        k = id(sem)
        if E.waited.get(k, (None, 0))[1] >= val:
            return
        E.waited[k] = (sem, val)
        E.eng.wait_ge(sem, val)

    def _deps(self, E, ins, outs):
        for a in ins:
            self._wait(E, self._r(a).w)
        for a in outs:
            r = self._r(a)
            self._wait(E, r.w)
            for tk in r.rs.values():
                self._wait(E, tk)

    def _commit(self, tk, ins, outs):
        for a in ins:
            self._r(a).rs[id(tk[0])] = tk
        for a in outs:
            r = self._r(a)
            r.w = tk
            r.rs = {}

    def op(self, en, fn, outs, ins):
        E = self.E[en]
        self._deps(E, ins, outs)
        inst = fn(E.eng)
        E.cnt += 1
        inst.then_inc(E.sem, 1)
        tk = (E.sem, E.cnt)
        self._commit(tk, ins, outs)
        return tk

    def dma(self, q, out, in_, extra_ins=(), extra_outs=(), **kw):
        E = self.E[q]
        sl = self.slots[q][self.slot_i[q] % len(self.slots[q])]
        self.slot_i[q] += 1
        if sl[1] > 0:
            self._wait(E, (sl[0], sl[1]))
        ins = [in_] + list(extra_ins)
        outs = [out] + list(extra_outs)
        self._deps(E, ins, outs)
        inst = E.eng.dma_start(out=out, in_=in_, **kw)
        sl[1] += 16
        inst.then_inc(sl[0], 16)
        tk = (sl[0], sl[1])
        self._commit(tk, ins, outs)
        return tk

    def mm(self, out, lhsT, rhs, start=True, stop=True, **kw):
        return self.op("tensor", lambda e: e.matmul(out, lhsT=lhsT, rhs=rhs, start=start, stop=stop, **kw), [out], [lhsT, rhs])

    def tr(self, out, in_, ident):
        return self.op("tensor", lambda e: e.transpose(out, in_, ident), [out], [in_, ident])

    def act(self, out, in_, func, scale=1.0, bias=0.0, eng="scalar"):
        ins = [in_] + [a for a in (scale, bias) if not isinstance(a, (int, float))]
        return self.op("scalar", lambda e: e.activation(out=out, in_=in_, func=func, scale=scale, bias=bias), [out], ins)

    def tt(self, out, in0, in1, op, eng="vector"):
        return self.op(eng, lambda e: e.tensor_tensor(out=out, in0=in0, in1=in1, op=op), [out], [in0, in1])

    def ts(self, out, in0, s1, op0, s2=None, op1=None, eng="vector"):
        ins = [in0] + [a for a in (s1, s2) if a is not None and not isinstance(a, (int, float))]
        if op1 is None:
            return self.op(eng, lambda e: e.tensor_scalar(out=out, in0=in0, scalar1=s1, scalar2=None, op0=op0), [out], ins)
        return self.op(eng, lambda e: e.tensor_scalar(out=out, in0=in0, scalar1=s1, scalar2=s2, op0=op0, op1=op1), [out], ins)

    def stt(self, out, in0, scalar, in1, op0, op1):
        ins = [in0, in1] + ([] if isinstance(scalar, (int, float)) else [scalar])
        return self.op("vector", lambda e: e.scalar_tensor_tensor(out=out, in0=in0, scalar=scalar, in1=in1, op0=op0, op1=op1), [out], ins)

    def cp(self, out, in_, eng="vector"):
        if eng == "scalar":
            return self.act(out, in_, AF.Copy)
        return self.op(eng, lambda e: e.tensor_copy(out=out, in_=in_), [out], [in_])

    def memset(self, ap, val, eng="vector"):
        return self.op(eng, lambda e: e.memset(ap, val), [ap], [])

    def finish(self):
        E = self.E["sync"]
        for q in self.slots:
            for sl in self.slots[q]:
                if sl[1] > 0:
                    self._wait(E, (sl[0], sl[1]))
        for n, e2 in self.E.items():
            if n != "sync" and e2.cnt > 0:
                self._wait(E, (e2.sem, e2.cnt))


def _bf(a):
    return np.ascontiguousarray(a.astype(ml_dtypes.bfloat16))


def dft_mats(Ls):
    N = 2 * Ls
    s = np.arange(Ls, dtype=np.int64)[:, None]
    f = np.arange(Ls, dtype=np.int64)[None, :]
    ang = 2.0 * np.pi * ((s * f) % N).astype(np.float64) / N
    Fm = np.empty((Ls, N), np.float64)
    Fm[:, :Ls] = np.cos(ang)
    Fm[:, Ls:] = np.sin(ang)
    Fm[:, Ls] = np.cos(np.pi * s[:, 0])
    nst, nft = Ls // 128, N // 128
    nblk = nft // 4
    half = nft // 2
    Fh = np.empty((nblk, 128, nst, 512), np.float32)
    Gh = np.empty((nblk, 128, 4, Ls), np.float32)
    for b in range(nblk):
        tiles = [2 * b, 2 * b + 1, half + 2 * b, half + 2 * b + 1]
        for i, ft in enumerate(tiles):
            blk = Fm[:, ft * 128:(ft + 1) * 128]
            Fh[b, :, :, i * 128:(i + 1) * 128] = blk.reshape(nst, 128, 128).transpose(1, 0, 2)
            Gh[b, :, i, :] = blk.T
    return _bf(Fh.reshape(nblk, 128, nst * 512)), _bf(Gh.reshape(nblk, 128, 4 * Ls))


def hy_pos_feats(Ls):
    t01 = np.linspace(0.0, 1.0, Ls, dtype=np.float32)[:, None]
    ang = (2.0 * np.pi * np.arange(Ls, dtype=np.float32)[:, None] / Ls).astype(np.float32)
    bands = np.linspace(1e-4, 15, 16, dtype=np.float32)[None, :]
    z = np.concatenate([t01, np.cos(bands * ang), -np.sin(bands * ang)], axis=-1).astype(np.float32)
    deltas = np.abs(np.linspace(math.log(1e-2) / 1.5, math.log(1e-2) / 0.3, 1024, dtype=np.float32))
    dec = np.exp(-t01 * deltas[None, :]).astype(np.float32)
    return np.ascontiguousarray(z.T), dec


def make_consts():
    c = {}
    c["ident_f"] = np.eye(128, dtype=np.float32)
    c["ident_b"] = _bf(np.eye(128, dtype=np.float32))
    c["Fh2048"], c["Gh2048"] = dft_mats(2048)
    c["Fh256"], c["Gh256"] = dft_mats(256)
    c["zT2048"], c["dec2048"] = hy_pos_feats(2048)
    c["zT256"], c["dec256"] = hy_pos_feats(256)
    inv = 1.0 / (10000.0 ** np.linspace(0.0, 1.0, 128, dtype=np.float32))
    ang = (np.arange(T, dtype=np.float32)[None, :] * inv[:, None].astype(np.float32)).astype(np.float32)
    c["rope"] = np.stack([np.cos(ang), np.sin(ang), np.cos(ang) / 16.0, np.sin(ang) / 16.0]).astype(np.float32)
    j = np.arange(8, dtype=np.float64)
    lg = np.log1p(-np.exp2(-5.0 - j))
    lgf, lgb = lg[0::2], lg[1::2]
    pos = np.arange(128, dtype=np.float64)
    rel = pos[None, :] - pos[:, None]
    rmask = np.zeros((4, 128, 128), np.float64)
    rq = np.zeros((4, 2, 128, 128), np.float64)
    rk = np.zeros((128, 4, 2), np.float64)
    for h in range(4):
        rmask[h] = np.where(rel >= 0, np.exp(np.maximum(rel, 0) * lgf[h]), 0.0) + np.where(rel <= 0, np.exp(np.maximum(-rel, 0) * lgb[h]), 0.0)
        rq[h, 0] = np.exp((pos + 1.0) * lgf[h])[None, :]
        rq[h, 1] = np.exp((128.0 - pos) * lgb[h])[None, :]
        rk[:, h, 0] = np.exp((127.0 - pos) * lgf[h])
        rk[:, h, 1] = np.exp(pos * lgb[h])
    c["rmask"] = rmask.astype(np.float32)
    c["rq"] = rq.astype(np.float32)
    c["rk"] = rk.astype(np.float32)
    c["rsdec"] = np.stack([np.exp(128.0 * lgf), np.exp(128.0 * lgb)]).astype(np.float32)
    same = (pos[:, None] // 32) == (pos[None, :] // 32)
    gm = np.zeros((2, 128, 128), np.float32)
    gm[0] = (same & (rel >= 0)).astype(np.float32)
    gm[1] = (same & (rel <= 0)).astype(np.float32)
    c["gmask"] = gm
    sm = np.ones((128, T), np.float32)
    sm[:, 0::32] = 0.0
    c["scanmask"] = sm
    sel = np.zeros((8, 8, 128), np.float32)
    for e in range(8):
        sel[e, e, :] = 1.0
    c["sel8"] = sel.reshape(8, 1024)
    c["ones_f"] = np.ones((128, 128), np.float32)
    c["ones_b"] = _bf(np.ones((128, 128), np.float32))
    return c


CONST_DT = {"ident_b": BF16, "Fh2048": BF16, "Gh2048": BF16, "Fh256": BF16, "Gh256": BF16, "ones_b": BF16}

W_SHAPES = {
    "ada_w": (DEPTH, D, 6 * D), "ada_b": (DEPTH, 6 * D), "w_in": (DEPTH, D, NCOL),
    "hy_conv_w": (DEPTH, 9, 3 * D), "hy_conv_b": (DEPTH, 3 * D),
    "hy_ff_w1": (DEPTH, 33, 64), "hy_ff_b1": (DEPTH, 64), "hy_ff_freq": (DEPTH, 2, 64),
    "hy_ff_w2": (DEPTH, 64, 64), "hy_ff_b2": (DEPTH, 64), "hy_ff_w3": (DEPTH, 64, 4096),
    "hy_skip": (DEPTH, 2, D), "hg_lb_logits": (2, DEPTH, D), "hg_norm_w": (DEPTH, D),
    "p_hy": (DEPTH, D, D), "p_hg": (DEPTH, D, D), "p_rt": (DEPTH, 2 * D, D), "w_o": (DEPTH, D, D),
    "ln1_g": (DEPTH, D), "ln1_b": (DEPTH, D), "ln2_g": (DEPTH, D), "ln2_b": (DEPTH, D),
    "ffn_w1": (1, D, D_FF), "ffn_w3": (1, D, D_FF), "ffn_w2": (1, D_FF, D),
    "moe_router": (1, D, NEXP), "moe_w1": (1, NEXP, D, D_EXP), "moe_w3": (1, NEXP, D, D_EXP),
    "moe_w2": (1, NEXP, D_EXP, D),
}


class Prog:
    def __init__(self, consts, stop_after=None, dbg=(), skip=()):
        self.nc = nc = bass.Bass("TRN2", target_bir_lowering=False)
        self.kb = kb = KB(nc)
        self.stop_after = stop_after
        self.dbg = set(dbg)
        kb.dbg = self.dbg
        self.ins = {}
        def inp(name, shape, dt=F32):
            self.ins[name] = nc.dram_tensor(name, list(shape), dt, kind="ExternalInput").ap()
            return self.ins[name]
        inp("x", (L, D)); inp("ctx", (LC, D)); inp("c2", (2, D))
        for n, s in W_SHAPES.items():
            if n not in skip:
                inp(n, s)
        for n, v in consts.items():
            inp(n, v.shape, CONST_DT.get(n, F32))
        self.out = nc.dram_tensor("out", [L, D], F32, kind="ExternalOutput").ap()
        self.dbg_out = {}

    def dbg_tensor(self, name, shape, dt=F32):
        kind = "ExternalOutput" if name in self.dbg else "Internal"
        t = self.nc.dram_tensor(name, list(shape), dt, kind=kind).ap()
        if name in self.dbg:
            self.dbg_out[name] = t
        return t

    def setup(self):
        kb, I = self.kb, self.ins
        self.ident_f = kb.sb([128, 128], F32); kb.dma("sync", self.ident_f[:], I["ident_f"][:, :])
        self.ident_b = kb.sb([128, 128], BF16); kb.dma("sync", self.ident_b[:], I["ident_b"][:, :])
        self.ones_f = kb.sb([128, 128], F32); kb.dma("sync", self.ones_f[:], I["ones_f"][:, :])
        self.ones_b = kb.sb([128, 128], BF16); kb.dma("sync", self.ones_b[:], I["ones_b"][:, :])
        self.eps_t = kb.sb([128, 1], F32); kb.memset(self.eps_t[:], EPS)
        self.xres = kb.dram("xres", [8, 128, T], F32)
        self.hT = kb.sb([128, 8, T], BF16, "hT")
        self.ybr = kb.dram("ybr", [4096, T], BF16)
        m = kb.mark()
        stg = [kb.sb([128, D], F32) for _ in range(2)]
        xo = [kb.sb([128, 8, 128], F32) for _ in range(2)]
        for n in range(NT):
            src = I["ctx"][n * 128:(n + 1) * 128, :] if n < 2 else I["x"][(n - 2) * 128:(n - 1) * 128, :]
            s = stg[n % 2]
            kb.dma("sync", s[:], src)
            for hb in range(2):
                ps = kb.ps()
                for j in range(4):
                    kt = hb * 4 + j
                    kb.tr(ps[:, j * 128:(j + 1) * 128], s[:, kt * 128:(kt + 1) * 128], self.ident_f[:])
                kb.cp(xo[n % 2][:, hb * 4:hb * 4 + 4, :],
                      ps[:].rearrange("p (j t) -> p j t", j=4), eng=("scalar" if hb else "vector"))
            kb.dma("sync", self.xres[:, :, n * 128:(n + 1) * 128].rearrange("k p t -> p k t"), xo[n % 2][:])
        kb.release(m)

    def load_rows_T(self, src2d, nrows, dst):
        kb = self.kb
        m = kb.mark()
        st = kb.sb([128, 128], F32)
        kb.dma("sync", st[0:nrows, :], src2d)
        ps = kb.ps()
        kb.tr(ps[:, 0:nrows], st[0:nrows, :], self.ident_f[0:nrows, 0:nrows])
        kb.cp(dst, ps[:, 0:nrows])
        kb.release(m)

    def modulation(self, l):
        kb, I = self.kb, self.ins
        self.mod = kb.sb([128, 48, 2], F32, "mod")
        self.sc1p = kb.sb([128, 8, 2], F32); self.sc2p = kb.sb([128, 8, 2], F32)
        m = kb.mark()
        c2 = kb.sb([2, D], F32)
        kb.dma("sync", c2[:], I["c2"][:, :])
        cs = kb.sb([2, D], F32)
        kb.act(cs[:], c2[:], AF.Silu)
        csT = kb.sb([128, 8, 2], F32)
        for kt in range(8):
            ps = kb.ps()
            kb.tr(ps[:, 0:2], cs[0:2, kt * 128:(kt + 1) * 128], self.ident_f[0:2, 0:2])
            kb.cp(csT[:, kt, :], ps[:, 0:2])
        ab = kb.sb([1, 6 * D], F32)
        kb.dma("sync", ab[:], I["ada_b"][l:l + 1, :])
        wb = [kb.sb([128, 8, 512], F32) for _ in range(2)]
        for cb in range(12):
            w = wb[cb % 2]
            kb.dma("sync", w[:], I["ada_w"][l, :, cb * 512:(cb + 1) * 512].rearrange("(k p) c -> p k c", p=128))
            ps = kb.ps()
            for j in range(4):
                o = ps[:, 2 * j:2 * j + 2]
                for kt in range(8):
                    kb.mm(o, w[:, kt, j * 128:(j + 1) * 128], csT[:, kt, :], start=(kt == 0), stop=False)
                jj = cb * 4 + j
                kb.mm(o, ab[0:1, jj * 128:(jj + 1) * 128], self.ones_f[0:1, 0:2], start=False, stop=True)
            kb.cp(self.mod[:, cb * 4:cb * 4 + 4, :], ps[:, 0:8].rearrange("p (j c) -> p j c", c=2))
        kb.ts(self.sc1p[:], self.mod[:, 8:16, :], 1.0, ALU.add)
        kb.ts(self.sc2p[:], self.mod[:, 32:40, :], 1.0, ALU.add)
        kb.release(m)
        self.sh1 = self.mod[:, 0:8, :]; self.gt1 = self.mod[:, 16:24, :]
        self.sh2 = self.mod[:, 24:32, :]; self.gt2 = self.mod[:, 40:48, :]

    def tok_chunks(self, t0, t1, step=512):
        out = []
        for a, b, col in ((0, LC, 1), (LC, T, 0)):
            a, b = max(a, t0), min(b, t1)
            while a < b:
                n = min(step, b - a)
                out.append((a, n, col))
                a += n
        return out

    def modulate(self, scp, sh, tok0=0):
        kb = self.kb
        m = kb.mark()
        xb = [kb.sb([128, 8, 512], F32) for _ in range(2)]
        for i, (t0, n, col) in enumerate(self.tok_chunks(tok0, T)):
            x = xb[i % 2]
            kb.dma("sync", x[:, :, 0:n], self.xres[:, :, t0:t0 + n].rearrange("k p t -> p k t"))
            for kt in range(8):
                kb.act(self.hT[:, kt, t0:t0 + n], x[:, kt, 0:n], AF.Identity, scale=scp[:, kt, col:col + 1], bias=sh[:, kt, col:col + 1])
        kb.release(m)

    def load_w(self, dst, src2d, q="gpsimd"):
        v = src2d.rearrange("(k p) c -> p k c", p=128)
        nk = v.shape[1]
        step = max(1, 1024 // 128)
        for k0 in range(0, nk, step):
            k1 = min(nk, k0 + step)
            self.kb.dma(q, dst[:, k0:k1, :], v[:, k0:k1, :])

    def proj_fm(self, w, j, tok0, ntok, nk=8, rhsT=None):
        kb = self.kb
        rhsT = self.hT if rhsT is None else rhsT
        t = tok0
        while t < tok0 + ntok:
            n = min(512, tok0 + ntok - t)
            ps = kb.ps()
            for kt in range(nk):
                kb.mm(ps[:, 0:n], w[:, kt, j * 128:(j + 1) * 128], rhsT[:, kt, t:t + n], start=(kt == 0), stop=(kt == nk - 1))
            yield ps, t, n
            t += n

    def retention(self, l, ctx_out):
        kb, I = self.kb, self.ins
        W = I["w_in"][l]
        m0 = kb.mark()
        rope = kb.sb([128, 2, T], F32, "rope")
        kb.dma("sync", rope[:], I["rope"][0:2].rearrange("f p t -> p f t"))
        rmask = kb.sb([128, 4, 128], F32); kb.dma("sync", rmask[:], I["rmask"].rearrange("h s t -> s h t"))
        rq = kb.sb([128, 8, 128], F32); kb.dma("sync", rq[:], I["rq"].rearrange("h d p t -> p (h d) t"))
        rk = kb.sb([128, 8], F32); kb.dma("sync", rk[:], I["rk"].rearrange("s h d -> s (h d)"))
        j8 = np.arange(8, dtype=np.float64)
        lg = np.log1p(-np.exp2(-5.0 - j8))
        sdec = [[float(np.exp(128.0 * lg[2 * h + d])) for d in range(2)] for h in range(4)]
        tok_out0 = 0 if ctx_out else LC
        tiles_out = list(range(0 if ctx_out else 2, NT))
        for hd in range(4):
            m1 = kb.mark()
            qrT = kb.sb([128, 2, T], BF16); krT = kb.sb([128, 2, T], BF16)
            vtm = kb.sb([128, NT, 512], BF16)
            kh = [kb.sb([128, NT, 256], BF16) for _ in range(2)]
            oT = kb.sb([128, 4, T], BF16)
            S = [[kb.sb([128, 512], F32) for _ in range(2)] for _ in range(2)]
            Sb = [[kb.sb([128, 512], BF16) for _ in range(2)] for _ in range(2)]
            scT = kb.sb([128, 128], BF16)
            qd = [kb.sb([128, 2, 128], BF16) for _ in range(2)]
            m2 = kb.mark()
            wq = kb.sb([128, 8, 256], BF16); self.load_w(wq[:], W[:, C_RQ + hd * 256:C_RQ + (hd + 1) * 256])
            wk = kb.sb([128, 8, 256], BF16); self.load_w(wk[:], W[:, C_RK + hd * 256:C_RK + (hd + 1) * 256])
            wv = kb.sb([128, 8, 512], BF16); self.load_w(wv[:], W[:, C_RV + hd * 512:C_RV + (hd + 1) * 512])
            raw = [kb.sb([128, 512], F32) for _ in range(2)]
            ta = kb.sb([128, 512], F32); tb = kb.sb([128, 512], F32)
            for (w, dst, sc) in ((wq, qrT, 1.0), (wk, krT, 1.0 / 16.0)):
                t0 = 0
                while t0 < T:
                    n = min(512, T - t0)
                    for j in range(2):
                        ps = kb.ps()
                        for kt in range(8):
                            kb.mm(ps[:, 0:n], w[:, kt, j * 128:(j + 1) * 128], self.hT[:, kt, t0:t0 + n], start=(kt == 0), stop=(kt == 7))
                        kb.act(raw[j][:, 0:n], ps[:, 0:n], AF.Copy, scale=sc)
                    cosT, sinT = rope[:, 0, t0:t0 + n], rope[:, 1, t0:t0 + n]
                    kb.tt(ta[:, 0:n], raw[0][:, 0:n], cosT, ALU.mult); kb.tt(tb[:, 0:n], raw[1][:, 0:n], sinT, ALU.mult)
                    kb.tt(dst[:, 0, t0:t0 + n], ta[:, 0:n], tb[:, 0:n], ALU.subtract)
                    kb.tt(ta[:, 0:n], raw[0][:, 0:n], sinT, ALU.mult); kb.tt(tb[:, 0:n], raw[1][:, 0:n], cosT, ALU.mult)
                    kb.tt(dst[:, 1, t0:t0 + n], ta[:, 0:n], tb[:, 0:n], ALU.add)
                    t0 += n
            for n in range(NT):
                ps = kb.ps()
                for kt in range(8):
                    kb.mm(ps[:], self.hT[:, kt, n * 128:(n + 1) * 128], wv[:, kt, :], start=(kt == 0), stop=(kt == 7))
                kb.cp(vtm[:, n, :], ps[:], eng="scalar")
                pt = kb.ps()
                ptb = pt[:].bitcast(BF16)
                for kt in range(2):
                    kb.tr(ptb[:, kt * 128:(kt + 1) * 128], krT[:, kt, n * 128:(n + 1) * 128], self.ident_b[:])
                for d in range(2):
                    kb.ts(kh[d][:, n, :], ptb[:, 0:256], rk[:, hd * 2 + d:hd * 2 + d + 1], ALU.mult)
            kb.release(m2)
            for d in range(2):
                for kt in range(2):
                    kb.memset(S[d][kt][:], 0.0); kb.memset(Sb[d][kt][:], 0.0)
            for d in range(2):
                order = list(range(NT)) if d == 0 else [1, 0] + list(range(NT - 1, 1, -1))
                for n in order:
                    tsl = slice(n * 128, (n + 1) * 128)
                    if n in tiles_out:
                        q_ = qd[n % 2]
                        kb.tt(q_[:], qrT[:, :, tsl], rq[:, hd * 2 + d, :].unsqueeze(1).broadcast_to([128, 2, 128]), ALU.mult)
                        po = kb.ps()
                        if d == 0:
                            psc = kb.ps()
                            for kt in range(2):
                                kb.mm(psc[:, 0:128], krT[:, kt, tsl], qrT[:, kt, tsl], start=(kt == 0), stop=(kt == 1))
                            kb.tt(scT[:], psc[:, 0:128], rmask[:, hd, :], ALU.mult)
                        for j in range(4):
                            o = po[:, j * 128:(j + 1) * 128]
                            if d == 0:
                                kb.mm(o, vtm[:, n, j * 128:(j + 1) * 128], scT[:], start=True, stop=False)
                            for kt in range(2):
                                kb.mm(o, Sb[d][kt][:, j * 128:(j + 1) * 128], q_[:, kt, :],
                                      start=(d == 1 and kt == 0), stop=(kt == 1))
                        ov = oT[:, :, tsl]
                        pv = po[:].rearrange("p (j t) -> p j t", j=4)
                        if d == 0:
                            kb.cp(ov, pv, eng="scalar")
                        else:
                            kb.tt(ov, ov, pv, ALU.add)
                    for kt in range(2):
                        pS = kb.ps()
                        kb.mm(pS[:], kh[d][:, n, kt * 128:(kt + 1) * 128], vtm[:, n, :])
                        kb.stt(S[d][kt][:], S[d][kt][:], sdec[hd][d], pS[:], ALU.mult, ALU.add)
                        kb.cp(Sb[d][kt][:], S[d][kt][:], eng="scalar")
            wg = kb.sb([128, 8, 512], BF16); self.load_w(wg[:], W[:, C_RG + hd * 512:C_RG + (hd + 1) * 512])
            sq = kb.sb([128, 4, 512], BF16); rstd = kb.sb([128, 512], F32); gs = kb.sb([128, 512], F32)
            ybs = [kb.sb([128, 4, 512], BF16) for _ in range(2)]
            r0 = 2048 + hd * 512
            t0 = tok_out0
            ci = 0
            while t0 < T:
                n = min(512, T - t0)
                yb = ybs[ci % 2]; ci += 1
                kb.act(sq[:, :, 0:n], oT[:, :, t0:t0 + n], AF.Square)
                pq = kb.ps()
                for j in range(4):
                    kb.mm(pq[:, 0:n], self.ones_b[:], sq[:, j, 0:n], start=(j == 0), stop=(j == 3))
                kb.act(rstd[:, 0:n], pq[:, 0:n], AF.Sqrt, scale=1.0 / 512.0, bias=self.eps_t[:])
                kb.op("vector", lambda e: e.reciprocal(out=rstd[:, 0:n], in_=rstd[:, 0:n]), [rstd[:]], [rstd[:]])
                for j in range(4):
                    pg = kb.ps()
                    for kt in range(8):
                        kb.mm(pg[:, 0:n], wg[:, kt, j * 128:(j + 1) * 128], self.hT[:, kt, t0:t0 + n], start=(kt == 0), stop=(kt == 7))
                    kb.act(gs[:, 0:n], pg[:, 0:n], AF.Silu)
                    kb.tt(gs[:, 0:n], gs[:, 0:n], rstd[:, 0:n], ALU.mult)
                    kb.tt(yb[:, j, 0:n], oT[:, j, t0:t0 + n], gs[:, 0:n], ALU.mult)
                kb.dma("sync", self.ybr[r0:r0 + 512, t0:t0 + n].rearrange("(j p) t -> p j t", p=128), yb[:, :, 0:n])
                t0 += n
            kb.release(m1)
        kb.release(m0)


    def hgrn2(self, l, ctx_out):
        kb, I = self.kb, self.ins
        W = I["w_in"][l]
        m0 = kb.mark()
        gmask = kb.sb([128, 2, 128], F32); kb.dma("sync", gmask[:], I["gmask"].rearrange("d s t -> s d t"))
        smask = kb.sb([128, T], F32); kb.dma("sync", smask[:], I["scanmask"][:, :])
        lbl = kb.sb([128, 32], F32)
        self.load_rows_T(I["hg_lb_logits"].rearrange("d l (k p) -> (d l k) p", p=128), 32, lbl[:])
        lb = kb.sb([128, 2, 8], F32); oml = kb.sb([128, 2, 8], F32)
        lv = lbl[:].rearrange("p (d l k) -> p d l k", d=2, l=2)
        if l == 0:
            kb.memset(lb[:], 0.0)
        else:
            kb.tt(lb[:], lv[:, :, 1, :], lv[:, :, 0, :], ALU.subtract)
            kb.act(lb[:], lb[:], AF.Sigmoid)
        kb.ts(oml[:], lb[:], -1.0, ALU.mult, 1.0, ALU.add)
        nw = kb.sb([128, 8], F32)
        self.load_rows_T(I["hg_norm_w"][l].rearrange("(k p) -> k p", p=128), 8, nw[:])
        tok_out0 = 0 if ctx_out else LC
        tiles_out = list(range(0 if ctx_out else 2, NT))
        NCH = T // 32
        for hd in range(8):
            m1 = kb.mark()
            qf = kb.sb([128, T], BF16); kf = kb.sb([128, T], BF16); qbi = kb.sb([128, T], BF16); kbk = kb.sb([128, T], BF16)
            qfo = kb.sb([128, T], F32); qbo = kb.sb([128, T], F32)
            khat = [kb.sb([128, NT, 128], BF16) for _ in range(2)]
            khz = [kb.sb([128, NT, 128], BF16) for _ in range(2)]
            vtm = kb.sb([128, NT, 128], BF16)
            etot = [kb.sb([128, NCH], F32) for _ in range(2)]
            oacc = kb.sb([128, T], F32)
            SS = [kb.sb([128, 128], F32) for _ in range(2)]
            scT = kb.sb([128, 128], BF16); scF = kb.sb([128, 128], F32)
            scT2 = kb.sb([128, 128], BF16); scF2 = kb.sb([128, 128], F32)
            SS2 = [kb.sb([128, 128], F32) for _ in range(2)]
            m2 = kb.mark()
            ws = {}
            for nm, c0 in (("ff", C_FF), ("fb", C_FB), ("q", C_HQ), ("v", C_HI)):
                ws[nm] = kb.sb([128, 8, 128], BF16)
                self.load_w(ws[nm][:], W[:, c0 + hd * 128:c0 + (hd + 1) * 128])
            tA = kb.sb([128, T], F32); tB = kb.sb([128, T], F32); tC = kb.sb([128, T], F32); tD = kb.sb([128, T], F32)
            tQ = kb.sb([128, T], F32); tK = kb.sb([128, T], BF16); tE = kb.sb([128, T], F32); totv = kb.sb([128, T // 32], F32)
            for ps, t0, n in self.proj_fm(ws["q"], 0, 0, T):
                kb.act(tQ[:, t0:t0 + n], ps[:, 0:n], AF.Silu)
            kb.ts(tQ[:], tQ[:], 128.0 ** -0.5, ALU.mult)
            for n in range(NT):
                ps = kb.ps()
                for kt in range(8):
                    kb.mm(ps[:, 0:128], self.hT[:, kt, n * 128:(n + 1) * 128], ws["v"][:, kt, :], start=(kt == 0), stop=(kt == 7))
                kb.cp(vtm[:, n, :], ps[:, 0:128], eng="scalar")
            for d in range(2):
                for ps, t0, n in self.proj_fm(ws["ff" if d == 0 else "fb"], 0, 0, T):
                    kb.act(tA[:, t0:t0 + n], ps[:, 0:n], AF.Sigmoid)
                kb.ts(tA[:], tA[:], oml[:, d, hd:hd + 1], ALU.mult, lb[:, d, hd:hd + 1], ALU.add)
                kb.act(tB[:], tA[:], AF.Ln)
                kb.ts(tA[:], tA[:], -1.0, ALU.mult, 1.0, ALU.add)
                kb.op("vector", lambda e: e.tensor_tensor_scan(out=tC[:], data0=smask[:], data1=tB[:], initial=0.0, op0=ALU.mult, op1=ALU.add),
                      [tC[:]], [smask[:], tB[:]])
                kb.act(etot[d][:], tC[:, 31::32], AF.Exp)
                kb.cp(totv[:], tC[:, 31::32])
                v3 = lambda a: a.rearrange("p (c k) -> p c k", k=32)
                bc = lambda a: a.unsqueeze(2).broadcast_to([128, NCH, 32])
                tb_ = bc(totv[:])
                if d == 0:
                    kb.tt(v3(tD[:]), v3(tC[:]), bc(tC[:, 15::32]), ALU.subtract)
                    kb.ts(tD[:], tD[:], 80.0, ALU.min, -80.0, ALU.max)
                    kb.act(tE[:], tD[:], AF.Exp)
                    kb.tt(qf[:], tQ[:], tE[:], ALU.mult)
                    kb.act(tE[:], tD[:], AF.Exp, scale=-1.0)
                    kb.tt(kf[:], tA[:], tE[:], ALU.mult)
                    kb.act(tE[:], tC[:], AF.Exp)
                    kb.tt(qfo[:], tQ[:], tE[:], ALU.mult)
                    kb.tt(v3(tD[:]), tb_, v3(tC[:]), ALU.subtract)
                    kb.act(tE[:], tD[:], AF.Exp)
                    kb.tt(tK[:], tA[:], tE[:], ALU.mult)
                else:
                    kb.tt(tC[:], tC[:], tB[:], ALU.subtract)
                    kb.tt(v3(tD[:]), v3(tC[:]), bc(tC[:, 16::32]), ALU.subtract)
                    kb.ts(tD[:], tD[:], 80.0, ALU.min, -80.0, ALU.max)
                    kb.act(tE[:], tD[:], AF.Exp)
                    kb.tt(kbk[:], tA[:], tE[:], ALU.mult)
                    kb.act(tE[:], tD[:], AF.Exp, scale=-1.0)
                    kb.tt(qbi[:], tQ[:], tE[:], ALU.mult)
                    kb.tt(v3(tD[:]), tb_, v3(tC[:]), ALU.subtract)
                    kb.act(tE[:], tD[:], AF.Exp)
                    kb.tt(qbo[:], tQ[:], tE[:], ALU.mult)
                    kb.act(tE[:], tC[:], AF.Exp)
                    kb.tt(tK[:], tA[:], tE[:], ALU.mult)
                for n in range(NT):
                    pt = kb.ps()
                    ptb = pt[:].bitcast(BF16)
                    kb.tr(ptb[:, 0:128], tK[:, n * 128:(n + 1) * 128], self.ident_b[:])
                    kb.cp(khat[d][:, n, :], ptb[:, 0:128], eng="scalar")
                kb.cp(khz[d][64:128, :, :], khat[d][64:128, :, :], eng="gpsimd")
                kb.memset(khz[d][64:96, :, :], 0.0, eng="gpsimd")
            kb.release(m2)
            SSd = [SS, SS2]
            scTd = [scT, scT2]; scFd = [scF, scF2]
            cur = [0, 0]
            for d in range(2):
                kb.memset(SSd[d][0][:], 0.0)
            orders = [list(range(NT)), [1, 0] + list(range(NT - 1, 1, -1))]
            QK = [(qf, kf, qfo), (qbi, kbk, qbo)]
            done = set()
            kb.ps_pool = [0, 1, 2, 3, 4, 5]
            for step in range(NT):
                ns = [orders[d][step] for d in range(2)]
                outs = [n in tiles_out for n in ns]
                pos = [kb.psum[6], kb.psum[7]]
                for d in range(2):
                    if outs[d]:
                        n = ns[d]
                        tsl = slice(n * 128, (n + 1) * 128)
                        psc = kb.ps()
                        kb.mm(psc[:, 0:128], QK[d][1][:, tsl], QK[d][0][:, tsl])
                        kb.ts(scFd[d][:], psc[:, 0:128], 1e30, ALU.min, -1e30, ALU.max)
                        kb.tt(scTd[d][:], scFd[d][:], gmask[:, d, :], ALU.mult, eng="gpsimd")
                        kb.mm(pos[d][:, 0:128], vtm[:, n, :], scTd[d][:], start=True, stop=False)
                for ci in range(4):
                    for d in range(2):
                        n = ns[d]
                        c = ci if d == 0 else 3 - ci
                        t32 = slice(n * 128 + c * 32, n * 128 + c * 32 + 32)
                        S_ = SSd[d]
                        if outs[d]:
                            kb.mm(pos[d][:, c * 32:c * 32 + 32], S_[cur[d]][:], QK[d][2][:, t32], start=False, stop=(ci == 3))
                        pS = kb.ps()
                        if c == 3:
                            kb.mm(pS[:, 0:128], khz[d][64:128, n, :], vtm[64:128, n, :])
                        else:
                            kb.mm(pS[:, 0:128], khat[d][c * 32:c * 32 + 32, n, :], vtm[c * 32:c * 32 + 32, n, :])
                        ch = n * 4 + c
                        kb.stt(S_[1 - cur[d]][:], S_[cur[d]][:], etot[d][:, ch:ch + 1], pS[:, 0:128], ALU.mult, ALU.add)
                        cur[d] = 1 - cur[d]
                for d in range(2):
                    if outs[d]:
                        n = ns[d]
                        tsl = slice(n * 128, (n + 1) * 128)
                        if n not in done:
                            kb.cp(oacc[:, tsl], pos[d][:, 0:128], eng="scalar")
                            done.add(n)
                        else:
                            kb.tt(oacc[:, tsl], oacc[:, tsl], pos[d][:, 0:128], ALU.add)
            kb.ps_pool = None
            wg = kb.sb([128, 8, 128], BF16); self.load_w(wg[:], W[:, C_HG + hd * 128:C_HG + (hd + 1) * 128])
            sq = kb.sb([128, 512], BF16); gs = kb.sb([128, 512], F32)
            ybs = [kb.sb([128, 512], BF16) for _ in range(2)]
            r0 = 1024 + hd * 128
            ci = 0
            for ps, t0, n in self.proj_fm(wg, 0, tok_out0, T - tok_out0):
                yb = ybs[ci % 2]; ci += 1
                kb.act(gs[:, 0:n], ps[:, 0:n], AF.Silu)
                kb.act(sq[:, 0:n], oacc[:, t0:t0 + n], AF.Square)
                pq = kb.ps()
                kb.mm(pq[:, 0:n], self.ones_b[:], sq[:, 0:n])
                if hd == 0:
                    kb.cp(self.ssq_hg[:, t0:t0 + n], pq[:, 0:n])
                else:
                    kb.tt(self.ssq_hg[:, t0:t0 + n], self.ssq_hg[:, t0:t0 + n], pq[:, 0:n], ALU.add)
                kb.stt(yb[:, 0:n], oacc[:, t0:t0 + n], nw[:, hd:hd + 1], gs[:, 0:n], ALU.mult, ALU.mult)
                kb.dma("sync", self.ybr[r0:r0 + 128, t0:t0 + n], yb[:, 0:n])
            kb.release(m1)
        kb.act(self.ssq_hg[:, tok_out0:T], self.ssq_hg[:, tok_out0:T], AF.Sqrt, scale=1.0 / 1024.0, bias=self.eps_t[:])
        kb.op("vector", lambda e: e.reciprocal(out=self.ssq_hg[:, tok_out0:T], in_=self.ssq_hg[:, tok_out0:T]), [self.ssq_hg[:]], [self.ssq_hg[:]])
        kb.release(m0)

    def hyena_filters(self, l, Ls, tag):
        kb, I = self.kb, self.ins
        nst, nblk = Ls // 128, (2 * Ls // 128) // 4
        N = 2 * Ls
        Kspec = kb.dram(f"kspec_{tag}_{l}", [2, 8, nblk, 128, 4, 256], F32)
        a_d = kb.dram(f"fa_{tag}_{l}", [Ls, 2048], BF16)
        b_d = kb.dram(f"fb_{tag}_{l}", [Ls, 2048], BF16)
        Fh = I[f"Fh{Ls}"]
        m0 = kb.mark()
        rn = kb.sb([128, 2048], F32)
        mA = kb.mark()
        w1 = kb.sb([33, 64], F32); kb.dma("sync", w1[:], I["hy_ff_w1"][l])
        w2 = kb.sb([64, 64], F32); kb.dma("sync", w2[:], I["hy_ff_w2"][l])
        w3 = kb.sb([64, 4096], F32); kb.dma("sync", w3[:], I["hy_ff_w3"][l])
        zT = kb.sb([33, Ls], F32); kb.dma("sync", zT[:], I[f"zT{Ls}"][:, :])
        st = kb.sb([4, 64], F32)
        kb.dma("sync", st[0:1, :], I["hy_ff_b1"][l:l + 1, :]); kb.dma("sync", st[1:2, :], I["hy_ff_b2"][l:l + 1, :])
        kb.dma("sync", st[2:4, :], I["hy_ff_freq"][l])
        pv = kb.sb([64, 4], F32)
        ps = kb.ps()
        kb.tr(ps[0:64, 0:4], st[0:4, :], self.ident_f[0:4, 0:4])
        kb.cp(pv[:], ps[0:64, 0:4])
        fb = kb.sb([64, 2], F32)
        kb.tt(fb[:, 0:1], pv[:, 0:1], pv[:, 2:3], ALU.mult); kb.tt(fb[:, 1:2], pv[:, 1:2], pv[:, 3:4], ALU.mult)
        hd1 = kb.sb([64, Ls], F32); hd2 = kb.sb([64, Ls], F32); wrapk = kb.sb([64, 512], F32)
        for (wm, kdim, src, dst, fi) in ((w1, 33, zT, hd1, 0), (w2, 64, hd1, hd2, 1)):
            t0 = 0
            while t0 < Ls:
                n = min(512, Ls - t0)
                ps = kb.ps()
                kb.mm(ps[0:64, 0:n], wm[0:kdim, :], src[0:kdim, t0:t0 + n])
                d_ = dst[:, t0:t0 + n]
                kb.act(d_, ps[0:64, 0:n], AF.Identity, scale=pv[:, 2 + fi:3 + fi], bias=fb[:, fi:fi + 1])
                k_ = wrapk[0:64, 0:n]
                kb.ts(k_, d_, 1.0 / TWO_PI, ALU.mult, 12582912.0, ALU.add)
                kb.ts(k_, k_, -12582912.0, ALU.add)
                kb.stt(d_, k_, -TWO_PI, d_, ALU.mult, ALU.add)
                kb.ts(d_, d_, math.pi, ALU.min, -math.pi, ALU.max)
                kb.act(d_, d_, AF.Sin)
                t0 += n
        kb.ps_pool = [0, 1, 2, 3]
        pn = [kb.psum[4 + i] for i in range(4)]
        hbuf = kb.sb([128, 8, 512], F32); habs = kb.sb([128, 8, 512], BF16)
        ab = [kb.sb([128, 2, 2048], BF16) for _ in range(2)]
        dec = [kb.sb([128, 1024], F32) for _ in range(2)]
        for tt_ in range(nst):
            dc = dec[tt_ % 2]
            kb.dma("sync", dc[:], I[f"dec{Ls}"][tt_ * 128:(tt_ + 1) * 128, :])
            for q in range(8):
                ps = kb.ps()
                kb.mm(ps[:], hd2[:, tt_ * 128:(tt_ + 1) * 128], w3[:, q * 512:(q + 1) * 512])
                kb.tt(hbuf[:, q, :], ps[:], dc[:, (q % 2) * 512:(q % 2) * 512 + 512], ALU.mult)
            if tt_ == 0:
                for q in (2, 3, 6, 7):
                    kb.memset(hbuf[0:1, q, :], 0.0)
            kb.act(habs[:], hbuf[:], AF.Abs)
            A_, B_ = ab[tt_ % 2][:, 0, :], ab[tt_ % 2][:, 1, :]
            for o in range(2):
                for hf in range(2):
                    csl = slice(o * 1024 + hf * 512, o * 1024 + hf * 512 + 512)
                    kb.tt(A_[:, csl], hbuf[:, o * 4 + hf, :], hbuf[:, o * 4 + 2 + hf, :], ALU.add, eng="gpsimd")
                    kb.tt(B_[:, csl], hbuf[:, o * 4 + hf, :], hbuf[:, o * 4 + 2 + hf, :], ALU.subtract, eng="gpsimd")
                    for dr in range(2):
                        kb.mm(pn[o * 2 + hf][:], self.ones_b[:], habs[:, o * 4 + dr * 2 + hf, :],
                              start=(tt_ == 0 and dr == 0), stop=(tt_ == nst - 1 and dr == 1))
            kb.dma("sync", a_d[tt_ * 128:(tt_ + 1) * 128, :], A_)
            kb.dma("sync", b_d[tt_ * 128:(tt_ + 1) * 128, :], B_)
        for i in range(4):
            kb.op("vector", lambda e: e.reciprocal(out=rn[:, i * 512:(i + 1) * 512], in_=pn[i][:]), [rn[:]], [pn[i][:]])
        kb.ts(rn[:], rn[:], 2.0 / N, ALU.mult)
        kb.ps_pool = None
        kb.release(mA)
        m1 = kb.mark()
        ach = kb.sb([128, nst, 512], BF16); bch = kb.sb([128, nst, 512], BF16)
        Fb = [kb.sb([128, nst, 512], BF16) for _ in range(2)]
        kq = [kb.sb([128, 4, 2, 512], F32) for _ in range(2)]
        for cc in range(4):
            o, ct0 = cc // 2, (cc % 2) * 4
            csl = slice(cc * 512, (cc + 1) * 512)
            av = a_d[:, csl].rearrange("(s p) c -> p s c", p=128); bv = b_d[:, csl].rearrange("(s p) c -> p s c", p=128)
            for k0 in range(0, nst, 8):
                k1 = min(nst, k0 + 8)
                kb.dma("sync", ach[:, k0:k1, :], av[:, k0:k1, :])
                kb.dma("sync", bch[:, k0:k1, :], bv[:, k0:k1, :])
            for blk in range(nblk):
                F_ = Fb[blk % 2]
                kb.dma("sync", F_[:], Fh[blk].rearrange("p (s f) -> p s f", f=512))
                K_ = kq[blk % 2]
                for i in range(4):
                    ps = kb.ps()
                    X = ach if i < 2 else bch
                    for s_ in range(nst):
                        kb.mm(ps[:], F_[:, s_, i * 128:(i + 1) * 128], X[:, s_, :], start=(s_ == 0), stop=(s_ == nst - 1))
                    if i < 2:
                        kb.tt(K_[:, 0, i, :], ps[:], rn[:, csl], ALU.mult)
                        kb.cp(K_[:, 3, i, :], K_[:, 0, i, :], eng="gpsimd")
                    else:
                        kb.tt(K_[:, 2, i - 2, :], ps[:], rn[:, csl], ALU.mult)
                        kb.ts(K_[:, 1, i - 2, :], K_[:, 2, i - 2, :], -1.0, ALU.mult, eng="gpsimd")
                if blk == 0:
                    ps = kb.ps()
                    for s_ in range(nst):
                        kb.mm(ps[0:1, :], F_[:, s_, 256:257], ach[:, s_, :], start=(s_ == 0), stop=(s_ == nst - 1))
                    kb.ts(K_[0:1, 0, 0, :], K_[0:1, 0, 0, :], 0.5, ALU.mult)
                    kb.memset(K_[0:1, 1, 0, :], 0.0); kb.memset(K_[0:1, 2, 0, :], 0.0)
                    kb.tt(K_[0:1, 3, 0, :], ps[0:1, :], rn[0:1, csl], ALU.mult)
                    kb.ts(K_[0:1, 3, 0, :], K_[0:1, 3, 0, :], 0.5, ALU.mult)
                for q in range(4):
                    for i in range(2):
                        kb.dma("sync", Kspec[o, ct0:ct0 + 4, blk, :, q, i * 128:(i + 1) * 128].rearrange("c p f -> p c f"),
                               K_[:, q, i, :].rearrange("p (c f) -> p c f", f=128))
        kb.release(m0)
        return Kspec

    def long_conv(self, z, o, ct, Ls, Kspec, bufs):
        kb, I = self.kb, self.ins
        nst, nblk = Ls // 128, (2 * Ls // 128) // 4
        Fh, Gh = I[f"Fh{Ls}"], I[f"Gh{Ls}"]
        zb, ztm, Yb, FG, Kq, U, tmp = bufs
        kb.cp(zb[:, 0:Ls], z, eng="gpsimd")
        for s0 in range(0, nst, 8):
            pt = kb.ps()
            ptb = pt[:].bitcast(BF16)
            k = min(8, nst - s0)
            for s_ in range(k):
                kb.tr(ptb[:, s_ * 128:(s_ + 1) * 128], zb[:, (s0 + s_) * 128:(s0 + s_ + 1) * 128], self.ident_b[:])
            kb.cp(ztm[:, s0:s0 + k, :], ptb[:, 0:k * 128].rearrange("p (s c) -> p s c", c=128), eng="scalar")
        for blk in range(nblk):
            F_ = FG[blk % 2]
            kb.dma("sync", F_[:, 0:nst * 512], Fh[blk])
            K_ = Kq[blk % 2]
            kb.dma("sync", K_[:], Kspec[o, ct, blk])
            pu = kb.ps()
            Fv = F_[:, 0:nst * 512].rearrange("p (s f) -> p s f", f=512)
            for i in range(4):
                for s_ in range(nst):
                    kb.mm(pu[:, i * 128:(i + 1) * 128], Fv[:, s_, i * 128:(i + 1) * 128], ztm[:, s_, :], start=(s_ == 0), stop=(s_ == nst - 1))
            kb.cp(U[:], pu[:], eng="scalar")
            Uc, Us = U[:, 0:256], U[:, 256:512]
            kb.tt(tmp[:, 0, :], Uc, K_[:, 0, :], ALU.mult); kb.tt(tmp[:, 1, :], Us, K_[:, 1, :], ALU.mult)
            kb.tt(Yb[:, blk, 0, :], tmp[:, 0, :], tmp[:, 1, :], ALU.add, eng="gpsimd")
            kb.tt(tmp[:, 2, :], Uc, K_[:, 2, :], ALU.mult); kb.tt(tmp[:, 3, :], Us, K_[:, 3, :], ALU.mult)
            kb.tt(Yb[:, blk, 1, :], tmp[:, 2, :], tmp[:, 3, :], ALU.add, eng="gpsimd")
        ntc = max(1, Ls // 512)
        py = [kb.psum[4 + i] for i in range(ntc)]
        for blk in range(nblk):
            G_ = FG[blk % 2]
            kb.dma("sync", G_[:, 0:4 * Ls], Gh[blk])
            Gv = G_[:, 0:4 * Ls].rearrange("p (i t) -> p i t", i=4)
            for i in range(4):
                lhs = Yb[:, blk, i // 2, (i % 2) * 128:(i % 2) * 128 + 128]
                for tc in range(ntc):
                    n = min(512, Ls)
                    kb.mm(py[tc][:, 0:n], lhs, Gv[:, i, tc * 512:tc * 512 + n], start=(blk == 0 and i == 0), stop=(blk == nblk - 1 and i == 3))
        return py

    def hyena(self, l, ctx_out, Ksp_lat, Ksp_ctx):
        kb, I = self.kb, self.ins
        W = I["w_in"][l]
        m0 = kb.mark()
        cwr = kb.sb([9, 3072], F32); kb.dma("sync", cwr[:], I["hy_conv_w"][l])
        cw = kb.sb([128, 24, 9], F32)
        for g in range(24):
            ps = kb.ps()
            kb.tr(ps[:, 0:9], cwr[0:9, g * 128:(g + 1) * 128], self.ident_f[0:9, 0:9])
            kb.cp(cw[:, g, :], ps[:, 0:9])
        cb = kb.sb([128, 24], F32)
        self.load_rows_T(I["hy_conv_b"][l].rearrange("(k p) -> k p", p=128), 24, cb[:])
        skp = kb.sb([128, 16], F32)
        self.load_rows_T(I["hy_skip"][l].rearrange("o (k p) -> (o k) p", p=128), 16, skp[:])
        upad = kb.sb([128, 34, 66], BF16); kb.memset(upad[:], 0.0)
        cpad = kb.sb([128, 258], BF16); kb.memset(cpad[:], 0.0)
        dg = kb.sb([128, 9, 128], BF16)
        zc = [kb.sb([128, T], F32) for _ in range(3)]
        z1 = kb.sb([128, T], F32)
        wb = [kb.sb([128, 8, 128], BF16) for _ in range(2)]
        zb = kb.sb([128, L], BF16); ztm = kb.sb([128, 16, 128], BF16); Yb = kb.sb([128, 8, 2, 256], BF16)
        FG = [kb.sb([128, 8192], BF16) for _ in range(2)]
        Kq = [kb.sb([128, 4, 256], F32) for _ in range(2)]
        U = kb.sb([128, 512], F32); tmp = kb.sb([128, 4, 256], F32)
        bufs = (zb, ztm, Yb, FG, Kq, U, tmp)
        yo = [kb.sb([128, 512], BF16) for _ in range(2)]
        kb.ps_pool = [0, 1, 2, 3]
        for ct in range(8):
            for g in range(3):
                gi = g * 8 + ct
                w = wb[g % 2]
                self.load_w(w[:], W[:, C_HY + gi * 128:C_HY + (gi + 1) * 128])
                for tap in range(9):
                    kb.ts(dg[:, tap, :], self.ident_b[:], cw[:, gi, tap:tap + 1], ALU.mult)
                if ctx_out:
                    for ps, t0, n in self.proj_fm(w, 0, 0, LC):
                        kb.cp(cpad[:, 1:257], ps[:, 0:256], eng="scalar")
                    ps2 = kb.ps()
                    for j in range(3):
                        kb.mm(ps2[:, 0:256], dg[:, 3 + j, :], cpad[:, j:j + 256], start=(j == 0), stop=(j == 2))
                    kb.act(zc[g][:, 0:LC], ps2[:, 0:256], AF.Identity, bias=cb[:, gi:gi + 1])
                ci = 0
                for ps, t0, n in self.proj_fm(w, 0, LC, L):
                    kb.cp(upad[:, 1 + 8 * ci:9 + 8 * ci, 1:65], ps[:].rearrange("p (r c) -> p r c", c=64), eng="scalar")
                    ci += 1
                for ci in range(4):
                    ps2 = kb.ps()
                    for tap in range(9):
                        dr, dc = tap // 3, tap % 3
                        kb.mm(ps2[:], dg[:, tap, :], upad[:, 8 * ci + dr:8 * ci + dr + 8, dc:dc + 64], start=(tap == 0), stop=(tap == 8))
                    kb.act(zc[g][:, LC + ci * 512:LC + (ci + 1) * 512], ps2[:], AF.Identity, bias=cb[:, gi:gi + 1])
            for (t0, Ls, Ksp) in (((0, LC, Ksp_ctx),) if ctx_out else ()) + ((LC, L, Ksp_lat),):
                zin = zc[0]
                for o in range(2):
                    py = self.long_conv(zin[:, t0:t0 + Ls], o, ct, Ls, Ksp, bufs)
                    gate = zc[1 + o]
                    for tc, p_ in enumerate(py):
                        n = min(512, Ls)
                        sl = slice(t0 + tc * 512, t0 + tc * 512 + n)
                        kb.stt(z1[:, sl], zin[:, sl], skp[:, o * 8 + ct:o * 8 + ct + 1], p_[:, 0:n], ALU.mult, ALU.add)
                        if o == 0:
                            kb.tt(z1[:, sl], z1[:, sl], gate[:, sl], ALU.mult)
                        else:
                            y_ = yo[tc % 2]
                            kb.tt(y_[:, 0:n], z1[:, sl], gate[:, sl], ALU.mult)
                            kb.dma("sync", self.ybr[ct * 128:(ct + 1) * 128, sl], y_[:, 0:n])
                    zin = z1
        kb.ps_pool = None
        kb.release(m0)

    def ln_stats(self, tp, n, bufs):
        kb = self.kb
        sqb, mean, rstd = bufs
        pm = kb.ps(); pq = kb.ps()
        for mt in range(8):
            kb.mm(pm[:, 0:n], self.ones_f[:], tp[:, mt, 0:n], start=(mt == 0), stop=(mt == 7))
        for mt in range(8):
            kb.act(sqb[:, 0:n], tp[:, mt, 0:n], AF.Square)
            kb.mm(pq[:, 0:n], self.ones_f[:], sqb[:, 0:n], start=(mt == 0), stop=(mt == 7))
        kb.ts(mean[:, 0:n], pm[:, 0:n], 1.0 / D, ALU.mult)
        kb.tt(rstd[:, 0:n], mean[:, 0:n], mean[:, 0:n], ALU.mult)
        kb.stt(rstd[:, 0:n], pq[:, 0:n], 1.0 / D, rstd[:, 0:n], ALU.mult, ALU.subtract)
        kb.act(rstd[:, 0:n], rstd[:, 0:n], AF.Sqrt, bias=self.eps_t[:])
        kb.op("vector", lambda e: e.reciprocal(out=rstd[:, 0:n], in_=rstd[:, 0:n]), [rstd[:]], [rstd[:]])
        return mean, rstd

    def ln_apply(self, tp, n, mean, rstd, lng, lnb):
        kb = self.kb
        for mt in range(8):
            kb.tt(tp[:, mt, 0:n], tp[:, mt, 0:n], mean[:, 0:n], ALU.subtract)
            kb.tt(tp[:, mt, 0:n], tp[:, mt, 0:n], rstd[:, 0:n], ALU.mult)
            kb.ts(tp[:, mt, 0:n], tp[:, mt, 0:n], lng[:, mt:mt + 1], ALU.mult, lnb[:, mt:mt + 1], ALU.add)

    def load_ln(self, l, which):
        kb, I = self.kb, self.ins
        g = kb.sb([128, 8], F32); b = kb.sb([128, 8], F32)
        self.load_rows_T(I[f"ln{which}_g"][l].rearrange("(k p) -> k p", p=128), 8, g[:])
        self.load_rows_T(I[f"ln{which}_b"][l].rearrange("(k p) -> k p", p=128), 8, b[:])
        return g, b

    def merge(self, l, ctx_out, moe):
        kb, I = self.kb, self.ins
        W = I["w_in"][l]
        m0 = kb.mark()
        lng, lnb = self.load_ln(l, 1)
        ych = kb.sb([128, 32, 512], BF16)
        macc = kb.sb([128, 8, 512], F32); mT = kb.sb([128, 8, 512], BF16)
        xc = kb.sb([128, 8, 512], F32); tp = kb.sb([128, 8, 512], F32)
        g_ = kb.sb([128, 512], F32); t_ = kb.sb([128, 512], F32)
        sqb = kb.sb([128, 512], F32); mean = kb.sb([128, 512], F32); rstd = kb.sb([128, 512], F32)
        wp = [kb.sb([128, 16, 512], BF16) for _ in range(2)]
        wg = [kb.sb([128, 8, 512], BF16) for _ in range(2)]
        if moe:
            rt = kb.sb([128, 8, 8], F32)
            kb.dma("sync", rt[:], I["moe_router"][0].rearrange("(k p) e -> p k e", p=128))
            lg = kb.sb([128, 8], F32); mx = kb.sb([128, 8], F32); g12 = kb.sb([128, 2], F32); wa = kb.sb([128, 8], F32); wb2 = kb.sb([128, 8], F32)
        P_ = (I["p_hy"][l], I["p_hg"][l], I["p_rt"][l])
        K0 = (0, 8, 16); NK = (8, 8, 16)
        wi = 0
        for (t0, n, col) in self.tok_chunks(0 if ctx_out else LC, T):
            yv = self.ybr[:, t0:t0 + n].rearrange("(k p) t -> p k t", p=128)
            for k0 in range(0, 32, 8):
                kb.dma("sync", ych[:, k0:k0 + 8, 0:n], yv[:, k0:k0 + 8, :])
            kb.dma("sync", xc[:, :, 0:n], self.xres[:, :, t0:t0 + n].rearrange("k p t -> p k t"))
            items = [(b, mh) for b in range(3) for mh in range(2)]

            def load_item(idx, slot):
                b_, mh_ = items[idx]
                self.load_w(wp[slot % 2][:, 0:NK[b_], :], P_[b_][:, mh_ * 512:(mh_ + 1) * 512])
                self.load_w(wg[slot % 2][:], W[:, C_BR + b_ * 1024 + mh_ * 512:C_BR + b_ * 1024 + (mh_ + 1) * 512])
            load_item(0, wi)
            for ii, (b, mh) in enumerate(items):
                w = wp[wi % 2]; w2 = wg[wi % 2]; wi += 1
                if ii + 1 < len(items):
                    load_item(ii + 1, wi)
                for m4 in range(4):
                    mt = mh * 4 + m4
                    msl = slice(m4 * 128, (m4 + 1) * 128)
                    ps = kb.ps()
                    for kt in range(NK[b]):
                        kb.mm(ps[:, 0:n], w[:, kt, msl], ych[:, K0[b] + kt, 0:n], start=(kt == 0), stop=(kt == NK[b] - 1))
                    pg = kb.ps()
                    for kt in range(8):
                        kb.mm(pg[:, 0:n], w2[:, kt, msl], self.hT[:, kt, t0:t0 + n], start=(kt == 0), stop=(kt == 7))
                    kb.act(g_[:, 0:n], pg[:, 0:n], AF.Sigmoid)
                    if b == 1:
                        kb.tt(g_[:, 0:n], g_[:, 0:n], self.ssq_hg[:, t0:t0 + n], ALU.mult)
                    if b == 0:
                        kb.tt(macc[:, mt, 0:n], ps[:, 0:n], g_[:, 0:n], ALU.mult)
                    else:
                        kb.tt(t_[:, 0:n], ps[:, 0:n], g_[:, 0:n], ALU.mult)
                        kb.tt(macc[:, mt, 0:n], macc[:, mt, 0:n], t_[:, 0:n], ALU.add)
            kb.cp(mT[:, :, 0:n], macc[:, :, 0:n], eng="scalar")
            self.load_w(wg[wi % 2][:], I["w_o"][l][:, 0:512])
            for mh in range(2):
                w = wg[wi % 2]; wi += 1
                if mh == 0:
                    self.load_w(wg[wi % 2][:], I["w_o"][l][:, 512:1024])
                for m4 in range(4):
                    mt = mh * 4 + m4
                    ps = kb.ps()
                    for kt in range(8):
                        kb.mm(ps[:, 0:n], w[:, kt, m4 * 128:(m4 + 1) * 128], mT[:, kt, 0:n], start=(kt == 0), stop=(kt == 7))
                    kb.act(xc[:, mt, 0:n], xc[:, mt, 0:n], AF.Copy, scale=ALPHA)
                    kb.stt(tp[:, mt, 0:n], ps[:, 0:n], self.gt1[:, mt, col:col + 1], xc[:, mt, 0:n], ALU.mult, ALU.add)
            mean_, rstd_ = self.ln_stats(tp, n, (sqb, mean, rstd))
            self.ln_apply(tp, n, mean_, rstd_, lng, lnb)
            kb.dma("sync", self.xres[:, :, t0:t0 + n].rearrange("k p t -> p k t"), tp[:, :, 0:n])
            for kt in range(8):
                kb.act(xc[:, kt, 0:n], tp[:, kt, 0:n], AF.Identity, scale=self.sc2p[:, kt, col:col + 1], bias=self.sh2[:, kt, col:col + 1])
            kb.cp(self.hT[:, :, t0:t0 + n], xc[:, :, 0:n], eng="scalar")
            if moe:
                for tt_ in range(n // 128):
                    pl = kb.ps()
                    for kt in range(8):
                        kb.mm(pl[:, 0:8], xc[:, kt, tt_ * 128:(tt_ + 1) * 128], rt[:, kt, :], start=(kt == 0), stop=(kt == 7))
                    kb.cp(lg[:], pl[:, 0:8])
                    kb.op("vector", lambda e: e.max(out=mx[:], in_=lg[:]), [mx[:]], [lg[:]])
                    kb.tt(g12[:, 0:1], mx[:, 0:1], mx[:, 1:2], ALU.subtract)
                    kb.act(g12[:, 1:2], g12[:, 0:1], AF.Sigmoid, scale=-1.0)
                    kb.act(g12[:, 0:1], g12[:, 0:1], AF.Sigmoid)
                    kb.ts(wa[:], lg[:], mx[:, 0:1], ALU.is_equal, g12[:, 0:1], ALU.mult)
                    kb.ts(wb2[:], lg[:], mx[:, 1:2], ALU.is_equal, g12[:, 1:2], ALU.mult)
                    kb.tt(wa[:], wa[:], wb2[:], ALU.add)
                    pt = kb.ps()
                    kb.tr(pt[0:8, 0:128], wa[:], self.ident_f[:])
                    kb.cp(self.gateT[0:8, t0 + tt_ * 128:t0 + (tt_ + 1) * 128], pt[0:8, 0:128])
        kb.release(m0)

    def ffn_dense(self, l):
        kb, I = self.kb, self.ins
        W1, W3, W2 = I["ffn_w1"][0], I["ffn_w3"][0], I["ffn_w2"][0]
        m0 = kb.mark()
        lng, lnb = self.load_ln(l, 2)
        hid = kb.sb([128, 22, 512], BF16)
        w1b = [kb.sb([128, 8, 512], BF16) for _ in range(2)]; w3b = [kb.sb([128, 8, 512], BF16) for _ in range(2)]
        w2t = [kb.sb([128, 22, 512], BF16) for _ in range(2)]
        xc = kb.sb([128, 8, 512], F32); tp = kb.sb([128, 8, 512], F32)
        s_ = [kb.sb([128, 512], F32) for _ in range(2)]
        sqb = kb.sb([128, 512], F32); mean = kb.sb([128, 512], F32); rstd = kb.sb([128, 512], F32)
        for (t0, n, col) in self.tok_chunks(0, T):
            kb.dma("sync", xc[:, :, 0:n], self.xres[:, :, t0:t0 + n].rearrange("k p t -> p k t"))
            def load_fblk(jb):
                nj_ = min(4, 22 - jb * 4)
                self.load_w(w1b[jb % 2][:, :, 0:nj_ * 128], W1[:, jb * 512:jb * 512 + nj_ * 128])
                self.load_w(w3b[jb % 2][:, :, 0:nj_ * 128], W3[:, jb * 512:jb * 512 + nj_ * 128])
            load_fblk(0)
            for jb in range(6):
                nj = min(4, 22 - jb * 4)
                a, b = w1b[jb % 2], w3b[jb % 2]
                if jb + 1 < 6:
                    load_fblk(jb + 1)
                else:
                    self.load_w(w2t[0][:], W2[:, 0:512])
                for j in range(nj):
                    p1 = kb.ps(); p3 = kb.ps()
                    for kt in range(8):
                        kb.mm(p1[:, 0:n], a[:, kt, j * 128:(j + 1) * 128], self.hT[:, kt, t0:t0 + n], start=(kt == 0), stop=(kt == 7))
                    for kt in range(8):
                        kb.mm(p3[:, 0:n], b[:, kt, j * 128:(j + 1) * 128], self.hT[:, kt, t0:t0 + n], start=(kt == 0), stop=(kt == 7))
                    sj = s_[j % 2]
                    kb.act(sj[:, 0:n], p1[:, 0:n], AF.Silu)
                    kb.tt(hid[:, jb * 4 + j, 0:n], sj[:, 0:n], p3[:, 0:n], ALU.mult)
            for mt in range(8):
                w = w2t[mt // 4]
                if mt == 0:
                    self.load_w(w2t[1][:], W2[:, 512:1024])
                ps = kb.ps()
                for j in range(22):
                    kb.mm(ps[:, 0:n], w[:, j, (mt % 4) * 128:(mt % 4 + 1) * 128], hid[:, j, 0:n], start=(j == 0), stop=(j == 21))
                kb.act(xc[:, mt, 0:n], xc[:, mt, 0:n], AF.Copy, scale=ALPHA)
                kb.stt(tp[:, mt, 0:n], ps[:, 0:n], self.gt2[:, mt, col:col + 1], xc[:, mt, 0:n], ALU.mult, ALU.add)
            mean_, rstd_ = self.ln_stats(tp, n, (sqb, mean, rstd))
            self.ln_apply(tp, n, mean_, rstd_, lng, lnb)
            kb.dma("sync", self.xres[:, :, t0:t0 + n].rearrange("k p t -> p k t"), tp[:, :, 0:n])
        kb.release(m0)

    def moe(self, l):
        kb, I = self.kb, self.ins
        m0 = kb.mark()
        lng, lnb = self.load_ln(l, 2)
        sel = kb.sb([8, 1024], F32); kb.dma("sync", sel[:], I["sel8"][:, :])
        hid = kb.sb([128, 28, 512], BF16)
        facc = kb.sb([128, 8, 512], F32)
        w1b = [kb.sb([128, 8, 512], BF16) for _ in range(2)]; w3b = [kb.sb([128, 8, 512], BF16) for _ in range(2)]
        w2h = [kb.sb([128, 28, 512], BF16)] * 2
        xc = kb.sb([128, 8, 512], F32)
        gb = kb.sb([128, 512], F32)
        s_ = [kb.sb([128, 512], F32) for _ in range(2)]
        sqb = kb.sb([128, 512], F32); mean = kb.sb([128, 512], F32); rstd = kb.sb([128, 512], F32)
        otm = [kb.sb([128, D], F32) for _ in range(2)]
        WE = [(I["moe_w1"][0, e], I["moe_w3"][0, e], I["moe_w2"][0, e]) for e in range(NEXP)]

        def load_blk(e, jb):
            self.load_w(w1b[jb % 2][:], WE[e][0][:, jb * 512:(jb + 1) * 512])
            self.load_w(w3b[jb % 2][:], WE[e][1][:, jb * 512:(jb + 1) * 512])

        chunks = self.tok_chunks(LC, T)
        for ci_, (t0, n, col) in enumerate(chunks):
            kb.dma("sync", xc[:, :, 0:n], self.xres[:, :, t0:t0 + n].rearrange("k p t -> p k t"))
            if ci_ == 0:
                load_blk(0, 0)
            for e in range(NEXP):
                W1, W3, W2 = WE[e]
                pb = kb.ps()
                kb.mm(pb[:, 0:n], sel[0:8, e * 128:(e + 1) * 128], self.gateT[0:8, t0:t0 + n])
                kb.cp(gb[:, 0:n], pb[:, 0:n], eng="scalar")
                w = w2h[0]
                self.load_w(w[:], W2[:, 0:512])
                for jb in range(7):
                    if jb + 1 < 7:
                        load_blk(e, jb + 1)
                    a, b = w1b[jb % 2], w3b[jb % 2]
                    for j in range(4):
                        p1 = kb.ps(); p3 = kb.ps()
                        for kt in range(8):
                            kb.mm(p1[:, 0:n], a[:, kt, j * 128:(j + 1) * 128], self.hT[:, kt, t0:t0 + n], start=(kt == 0), stop=(kt == 7))
                        for kt in range(8):
                            kb.mm(p3[:, 0:n], b[:, kt, j * 128:(j + 1) * 128], self.hT[:, kt, t0:t0 + n], start=(kt == 0), stop=(kt == 7))
                        sj = s_[j % 2]
                        kb.act(sj[:, 0:n], p1[:, 0:n], AF.Silu)
                        kb.tt(hid[:, jb * 4 + j, 0:n], sj[:, 0:n], p3[:, 0:n], ALU.mult)
                if e + 1 < NEXP:
                    load_blk(e + 1, 0)
                elif ci_ + 1 < len(chunks):
                    load_blk(0, 0)
                for hf in range(2):
                    if hf == 1:
                        self.load_w(w[:], W2[:, 512:1024])
                    for m4 in range(4):
                        mt = hf * 4 + m4
                        ps = kb.ps()
                        for j in range(28):
                            kb.mm(ps[:, 0:n], w[:, j, m4 * 128:(m4 + 1) * 128], hid[:, j, 0:n], start=(j == 0), stop=(j == 27))
                        if e == 0:
                            kb.tt(facc[:, mt, 0:n], ps[:, 0:n], gb[:, 0:n], ALU.mult)
                        else:
                            sj = s_[m4 % 2]
                            kb.tt(sj[:, 0:n], ps[:, 0:n], gb[:, 0:n], ALU.mult)
                            kb.tt(facc[:, mt, 0:n], facc[:, mt, 0:n], sj[:, 0:n], ALU.add)
            for mt in range(8):
                kb.act(xc[:, mt, 0:n], xc[:, mt, 0:n], AF.Copy, scale=ALPHA)
                kb.stt(facc[:, mt, 0:n], facc[:, mt, 0:n], self.gt2[:, mt, col:col + 1], xc[:, mt, 0:n], ALU.mult, ALU.add)
            mean_, rstd_ = self.ln_stats(facc, n, (sqb, mean, rstd))
            self.ln_apply(facc, n, mean_, rstd_, lng, lnb)
            for tt_ in range(n // 128):
                o_ = otm[tt_ % 2]
                for hb in range(2):
                    ps = kb.ps()
                    for j in range(4):
                        kb.tr(ps[:, j * 128:(j + 1) * 128], facc[:, hb * 4 + j, tt_ * 128:(tt_ + 1) * 128], self.ident_f[:])
                    kb.cp(o_[:, hb * 512:(hb + 1) * 512], ps[:], eng=("scalar" if hb else "vector"))
                r0 = t0 - LC + tt_ * 128
                kb.dma("sync", self.out[r0:r0 + 128, :], o_[:])
        kb.release(m0)

    def build_all(self, nlayers=2, stages=("ret", "hg", "hy", "merge", "ffn")):
        kb = self.kb
        self.setup()
        for l in range(nlayers):
            ctx_out = (l == 0)
            m = kb.mark()
            self.ssq_hg = kb.sb([128, T], F32, "ssq_hg")
            self.gateT = kb.sb([8, T], F32, "gateT")
            self.modulation(l)
            self.modulate(self.sc1p, self.sh1)
            Kl = self.hyena_filters(l, 2048, "lat") if "hy" in stages else None
            Kc = self.hyena_filters(l, 256, "ctx") if (ctx_out and "hy" in stages) else None
            if "ret" in stages:
                self.retention(l, ctx_out)
            if "hg" in stages:
                self.hgrn2(l, ctx_out)
            if "hy" in stages:
                self.hyena(l, ctx_out, Kl, Kc)
            if "merge" in stages:
                self.merge(l, ctx_out, moe=(l == 1))
            if "ffn" in stages:
                if l == 0:
                    self.ffn_dense(l)
                else:
                    self.moe(l)
            kb.release(m)
        self.finish()

    def finish(self, final_src=None):
        kb = self.kb
        kb.barrier()
        kb.finish()


def prep_inputs(inputs, consts):
    shared = {}
    for n, shp in W_SHAPES.items():
        shared[n] = np.ascontiguousarray(np.asarray(inputs[n], dtype=np.float32).reshape(shp))
    shared.update(consts)
    maps = []
    for b in range(8):
        m = dict(shared)
        m["x"] = np.ascontiguousarray(inputs["x"][b], dtype=np.float32)
        m["ctx"] = np.ascontiguousarray(inputs["ctx"][b], dtype=np.float32)
        m["c2"] = np.ascontiguousarray(np.stack([inputs["c"][b], inputs["c_ctx"]]), dtype=np.float32)
        maps.append(m)
    return maps


_CACHE = {}


def kernel(**inputs):
    if "c" not in _CACHE:
        _CACHE["c"] = make_consts()
    consts = _CACHE["c"]
    P = Prog(consts)
    P.build_all()
    maps = prep_inputs(inputs, consts)
    res = run_bass_kernel_spmd(P.nc, maps, core_ids=list(range(8)))
    return np.stack([np.asarray(r["out"], dtype=np.float32) for r in res.results], axis=0)
```
